# Optimizing a Trainium2 kernel written in Bass

```python
import math
import jax
import jax.numpy as jnp
from jax import lax
import numpy as np

D_MODEL = 1024
BATCH = 8
SEQ = 8192
DEPTH = 4

DN_HEADS = 8
DN_DK = 64
DN_DV = 64
DN_CHUNK = 64
CONV_W = 4
DN_QKV = DN_HEADS * (2 * DN_DK + DN_DV)
NSA_HEADS = 8
NSA_GROUPS = 2
NSA_HPG = NSA_HEADS // NSA_GROUPS
NSA_DH = 64
CMP_LEN = 32
CMP_STRIDE = 16
SEL_LEN = 64
SEL_TOP = 16
WINDOW = 512
NSA_Q_BLOCK = 64
ROPE_THETA = 10000.0
FFN_DIM = 3584
N_EXPERTS = 8
TOP_K = 2
EPS = 1e-6
NEG = -1e30
FORCE = 1e6

SPLIT_SIZES = (
    DN_QKV,
    DN_HEADS * DN_DV,
    DN_HEADS,
    DN_HEADS,
    NSA_HEADS * NSA_DH,
    NSA_GROUPS * NSA_DH,
    NSA_GROUPS * NSA_DH,
    NSA_GROUPS * NSA_DH,
    NSA_GROUPS * NSA_DH,
    NSA_GROUPS * NSA_DH,
    NSA_GROUPS * NSA_DH,
    3 * NSA_HEADS,
    2 * D_MODEL,
)
P_IN = sum(SPLIT_SIZES)

kernel_name = 'hybrid_deltanet_nsa_moe_adaln_trunk'


def rms_norm(x, w):
    xf = x.astype(jnp.float32)
    y = xf * lax.rsqrt(jnp.mean(xf * xf, -1, keepdims=True) + EPS)
    return (y * w.astype(jnp.float32)).astype(x.dtype)


def l2_norm(x):
    xf = x.astype(jnp.float32)
    return (xf * lax.rsqrt(jnp.sum(xf * xf, -1, keepdims=True) + EPS)).astype(x.dtype)


def rope_angles(pos):
    inv = 1.0 / (ROPE_THETA ** (jnp.arange(0, NSA_DH, 2, dtype=jnp.float32) / NSA_DH))
    ang = pos.astype(jnp.float32)[..., None] * inv
    return jnp.cos(ang)[:, :, None, :], jnp.sin(ang)[:, :, None, :]


def apply_rope(x, cos, sin):
    xf = x.astype(jnp.float32)
    x1, x2 = jnp.split(xf, 2, -1)
    return jnp.concatenate([x1 * cos - x2 * sin, x2 * cos + x1 * sin], -1).astype(x.dtype)


def masked_softmax(s, mask):
    s = jnp.where(mask, s.astype(jnp.float32), NEG)
    m = jnp.max(s, -1, keepdims=True)
    e = jnp.where(mask, jnp.exp(s - m), 0.0)
    return e / jnp.maximum(jnp.sum(e, -1, keepdims=True), 1e-30)


def causal_conv_silu(x, w):
    ch = x.shape[-1]
    y = lax.conv_general_dilated(x, w[:, None, :].astype(x.dtype), window_strides=(1,),
                                 padding=[(CONV_W - 1, 0)],
                                 dimension_numbers=('NWC', 'WIO', 'NWC'),
                                 feature_group_count=ch)
    return jax.nn.silu(y)


def gated_delta_rule(q, k, v, beta, g):
    b_, s_, h_, dk = q.shape
    dv = v.shape[-1]
    n, c = s_ // DN_CHUNK, DN_CHUNK
    f32 = jnp.float32
    qc = q.astype(f32).reshape(b_, n, c, h_, dk).transpose(1, 0, 3, 2, 4)
    kc = k.astype(f32).reshape(b_, n, c, h_, dk).transpose(1, 0, 3, 2, 4)
    vc = v.astype(f32).reshape(b_, n, c, h_, dv).transpose(1, 0, 3, 2, 4)
    bc = beta.astype(f32).reshape(b_, n, c, h_).transpose(1, 0, 3, 2)
    gc = jnp.cumsum(g.astype(f32).reshape(b_, n, c, h_).transpose(1, 0, 3, 2), axis=-1)
    tril = jnp.tril(jnp.ones((c, c), bool))
    strict = jnp.tril(jnp.ones((c, c), bool), -1)
    diff = gc[..., :, None] - gc[..., None, :]
    decay = jnp.where(tril, jnp.exp(jnp.where(tril, diff, 0.0)), 0.0)
    kb = kc * bc[..., None]
    lower = jnp.where(strict, jnp.einsum('nbhcd,nbhsd->nbhcs', kb, kc) * decay, 0.0)
    eye = jnp.eye(c, dtype=f32)
    tinv = lax.linalg.triangular_solve(eye + lower, jnp.broadcast_to(eye, lower.shape),
                                       left_side=True, lower=True, unit_diagonal=True)
    u = tinv @ (vc * bc[..., None])
    w = tinv @ (kb * jnp.exp(gc)[..., None])
    a_intra = jnp.where(tril, jnp.einsum('nbhcd,nbhsd->nbhcs', qc, kc) * decay, 0.0)
    q_dec = qc * jnp.exp(gc)[..., None]
    k_dec = kc * jnp.exp(gc[..., -1:] - gc)[..., None]
    g_last = jnp.exp(gc[..., -1])

    def step(state, xs):
        u_n, w_n, a_n, qd_n, kd_n, gl_n = xs
        v_new = u_n - w_n @ state
        o_n = qd_n @ state + a_n @ v_new
        state = state * gl_n[..., None, None] + jnp.swapaxes(kd_n, -1, -2) @ v_new
        return state, o_n

    s0 = jnp.zeros((b_, h_, dk, dv), f32)
    _, o = lax.scan(step, s0, (u, w, a_intra, q_dec, k_dec, g_last))
    return o.transpose(1, 0, 3, 2, 4).reshape(b_, s_, h_, dv).astype(q.dtype)


def compress_blocks(x, pos_emb, w1, w2):
    b_, s_ = x.shape[:2]
    n_cmp = (s_ - CMP_LEN) // CMP_STRIDE + 1
    idx = jnp.arange(n_cmp)[:, None] * CMP_STRIDE + jnp.arange(CMP_LEN)[None, :]
    blk = x[:, idx] + pos_emb[None, None, :, None, :]
    blk = blk.transpose(0, 1, 3, 2, 4).reshape(b_, n_cmp, NSA_GROUPS, CMP_LEN * NSA_DH)
    return jax.nn.silu(blk @ w1) @ w2


def nsa_core(q, k_c, v_c, k_s, v_s, k_w, v_w, gates):
    b_, s_ = q.shape[:2]
    n_cmp = k_c.shape[1]
    n_sel = s_ // SEL_LEN
    k_top = min(SEL_TOP, n_sel)
    qbk = NSA_Q_BLOCK
    scale = NSA_DH ** -0.5
    cmp_start = jnp.arange(n_cmp) * CMP_STRIDE
    cmp_end = cmp_start + CMP_LEN - 1
    blk_start = jnp.arange(n_sel) * SEL_LEN
    overlap = ((cmp_start[:, None] < blk_start[None, :] + SEL_LEN) &
               (cmp_start[:, None] + CMP_LEN > blk_start[None, :])).astype(jnp.float32)
    ks_blocks = k_s.reshape(b_, n_sel, SEL_LEN, NSA_GROUPS, NSA_DH).transpose(0, 3, 1, 2, 4)
    vs_blocks = v_s.reshape(b_, n_sel, SEL_LEN, NSA_GROUPS, NSA_DH).transpose(0, 3, 1, 2, 4)
    kw_pad = jnp.pad(k_w, ((0, 0), (WINDOW, 0), (0, 0), (0, 0)))
    vw_pad = jnp.pad(v_w, ((0, 0), (WINDOW, 0), (0, 0), (0, 0)))
    b_ix = jnp.arange(b_)[:, None, None, None]
    g_ix = jnp.arange(NSA_GROUPS)[None, :, None, None]
    jb = jnp.arange(n_sel)

    def block(s0):
        t = s0 + jnp.arange(qbk)
        qb = lax.dynamic_slice_in_dim(q, s0, qbk, 1).reshape(b_, qbk, NSA_GROUPS, NSA_HPG, NSA_DH) * scale
        s_c = jnp.einsum('bqghd,bcgd->bghqc', qb, k_c)
        p_c = masked_softmax(s_c, cmp_end[None, :] <= t[:, None])
        o_c = jnp.einsum('bghqc,bcgd->bqghd', p_c.astype(v_c.dtype), v_c)
        imp = jnp.einsum('bghqc,cj->bgqj', p_c, overlap)
        cur = t // SEL_LEN
        forced = (jb[None] == 0) | (jb[None] == cur[:, None]) | (jb[None] == cur[:, None] - 1)
        causal = blk_start[None] <= t[:, None]
        imp = jnp.where(forced, FORCE, jnp.where(causal, imp, -FORCE))
        _, sel = lax.top_k(imp, k_top)
        k_sel = ks_blocks[b_ix, g_ix, sel]
        v_sel = vs_blocks[b_ix, g_ix, sel]
        tok = sel[..., None] * SEL_LEN + jnp.arange(SEL_LEN)
        m_s = (tok <= t[:, None, None]).reshape(b_, NSA_GROUPS, 1, qbk, k_top * SEL_LEN)
        s_s = jnp.einsum('bqghd,bgqkld->bghqkl', qb, k_sel).reshape(
            b_, NSA_GROUPS, NSA_HPG, qbk, k_top * SEL_LEN)
        p_s = masked_softmax(s_s, m_s).reshape(b_, NSA_GROUPS, NSA_HPG, qbk, k_top, SEL_LEN)
        o_s = jnp.einsum('bghqkl,bgqkld->bqghd', p_s.astype(v_sel.dtype), v_sel)
        kw = lax.dynamic_slice_in_dim(kw_pad, s0, qbk + WINDOW, 1)
        vw = lax.dynamic_slice_in_dim(vw_pad, s0, qbk + WINDOW, 1)
        u = s0 - WINDOW + jnp.arange(qbk + WINDOW)
        dist = t[:, None] - u[None, :]
        m_w = (dist >= 0) & (dist < WINDOW) & (u[None, :] >= 0)
        s_w = jnp.einsum('bqghd,bkgd->bghqk', qb, kw)
        p_w = masked_softmax(s_w, m_w)
        o_w = jnp.einsum('bghqk,bkgd->bqghd', p_w.astype(vw.dtype), vw)
        gb = lax.dynamic_slice_in_dim(gates, s0, qbk, 1).reshape(b_, qbk, NSA_GROUPS, NSA_HPG, 3)
        o = gb[..., 0:1] * o_c + gb[..., 1:2] * o_s + gb[..., 2:3] * o_w
        return o.reshape(b_, qbk, NSA_HEADS * NSA_DH)

    out = lax.map(block, jnp.arange(s_ // qbk) * qbk)
    return out.transpose(1, 0, 2, 3).reshape(b_, s_, NSA_HEADS * NSA_DH)


def swiglu(h, w1, w3, w2):
    return (jax.nn.silu(h @ w1) * (h @ w3)) @ w2


def moe_ffn(h, w_router, w1, w3, w2):
    logits = (h @ w_router).astype(jnp.float32)
    top_val, top_idx = lax.top_k(logits, TOP_K)
    top_w = jax.nn.softmax(top_val, -1)
    combine = jnp.sum(jax.nn.one_hot(top_idx, N_EXPERTS, dtype=jnp.float32) * top_w[..., None], -2)
    out = jnp.zeros_like(h)
    for e in range(N_EXPERTS):
        out = out + combine[..., e:e + 1].astype(h.dtype) * swiglu(h, w1[e], w3[e], w2[e])
    return out


def setup_inputs(seed: int = 0) -> dict:
    key = jax.random.key(seed)
    keys = iter(jax.random.split(key, 40))

    def nrm(shape, scale):
        return jax.random.normal(next(keys), shape, jnp.float32) * scale

    L, D = DEPTH, D_MODEL
    n_dense, n_moe = (DEPTH + 1) // 2, DEPTH // 2
    x = nrm((BATCH, SEQ, D), 1.0)
    c = nrm((BATCH, D), 1.0)
    positions = (jnp.arange(SEQ, dtype=jnp.int32)[None, :] +
                 jax.random.randint(next(keys), (BATCH, 1), 0, 1024, dtype=jnp.int32))
    w_ada = nrm((L, D, 6 * D), 0.5 * D ** -0.5)
    b_ada = nrm((L, 6 * D), 0.01)
    norm_mix = 1.0 + nrm((L, D), 0.05)
    norm_ffn = 1.0 + nrm((L, D), 0.05)
    w_in = nrm((L, D, P_IN), D ** -0.5)
    conv_w = nrm((L, CONV_W, DN_QKV), CONV_W ** -0.5)
    a_log = jnp.log(jax.random.uniform(next(keys), (L, DN_HEADS), jnp.float32, 1.0, 16.0))
    dt = jnp.exp(jax.random.uniform(next(keys), (L, DN_HEADS), jnp.float32,
                                    math.log(1e-3), math.log(1e-1)))
    dt_bias = dt + jnp.log(-jnp.expm1(-dt))
    dn_norm = 1.0 + nrm((L, DN_DV), 0.05)
    cmp_pos = nrm((L, 2, CMP_LEN, NSA_DH), 0.1)
    w_cmp1 = nrm((L, 2, CMP_LEN * NSA_DH, NSA_DH), (CMP_LEN * NSA_DH) ** -0.5)
    w_cmp2 = nrm((L, 2, NSA_DH, NSA_DH), NSA_DH ** -0.5)
    q_norm = 1.0 + nrm((L, NSA_DH), 0.05)
    k_norm = 1.0 + nrm((L, 3, NSA_DH), 0.05)
    w_oa = nrm((L, DN_HEADS * DN_DV, D), (DN_HEADS * DN_DV) ** -0.5)
    w_ob = nrm((L, NSA_HEADS * NSA_DH, D), (NSA_HEADS * NSA_DH) ** -0.5)
    w_out = nrm((L, D, D), D ** -0.5)
    w1_dense = nrm((n_dense, D, FFN_DIM), D ** -0.5)
    w3_dense = nrm((n_dense, D, FFN_DIM), D ** -0.5)
    w2_dense = nrm((n_dense, FFN_DIM, D), FFN_DIM ** -0.5)
    w_router = nrm((n_moe, D, N_EXPERTS), D ** -0.5)
    w1_moe = nrm((n_moe, N_EXPERTS, D, FFN_DIM), D ** -0.5)
    w3_moe = nrm((n_moe, N_EXPERTS, D, FFN_DIM), D ** -0.5)
    w2_moe = nrm((n_moe, N_EXPERTS, FFN_DIM, D), FFN_DIM ** -0.5)
    return {'x': x, 'c': c, 'positions': positions, 'w_ada': w_ada, 'b_ada': b_ada,
            'norm_mix': norm_mix, 'norm_ffn': norm_ffn, 'w_in': w_in, 'conv_w': conv_w,
            'a_log': a_log, 'dt_bias': dt_bias, 'dn_norm': dn_norm, 'cmp_pos': cmp_pos,
            'w_cmp1': w_cmp1, 'w_cmp2': w_cmp2, 'q_norm': q_norm, 'k_norm': k_norm,
            'w_oa': w_oa, 'w_ob': w_ob, 'w_out': w_out, 'w1_dense': w1_dense,
            'w3_dense': w3_dense, 'w2_dense': w2_dense, 'w_router': w_router,
            'w1_moe': w1_moe, 'w3_moe': w3_moe, 'w2_moe': w2_moe}


def reference(x, c, positions, w_ada, b_ada, norm_mix, norm_ffn, w_in, conv_w, a_log, dt_bias,
              dn_norm, cmp_pos, w_cmp1, w_cmp2, q_norm, k_norm, w_oa, w_ob, w_out,
              w1_dense, w3_dense, w2_dense, w_router, w1_moe, w3_moe, w2_moe):
    b_, s_, _ = x.shape
    gq, dh = NSA_GROUPS, NSA_DH
    cos, sin = rope_angles(positions)
    n_cmp = (s_ - CMP_LEN) // CMP_STRIDE + 1
    cmp_end = jnp.arange(n_cmp) * CMP_STRIDE + CMP_LEN - 1
    cos_c, sin_c = rope_angles(positions[:, cmp_end])
    split_at = [int(i) for i in np.cumsum(SPLIT_SIZES)[:-1]]
    c_act = jax.nn.silu(c)
    for l in range(DEPTH):
        mod = c_act @ w_ada[l] + b_ada[l]
        sh_m, sc_m, g_m, sh_f, sc_f, g_f = [m[:, None, :] for m in jnp.split(mod, 6, -1)]
        h = rms_norm(x, norm_mix[l]) * (1.0 + sc_m) + sh_m
        proj = h @ w_in[l]
        (dn_qkv, dn_z, dn_b, dn_a, n_q, c_k, c_v, s_k, s_v, w_k, w_v, n_g, m_g) = \
            jnp.split(proj, split_at, -1)
        qkv = causal_conv_silu(dn_qkv, conv_w[l])
        d_q, d_k, d_v = jnp.split(qkv, [DN_HEADS * DN_DK, 2 * DN_HEADS * DN_DK], -1)
        d_q = l2_norm(d_q.reshape(b_, s_, DN_HEADS, DN_DK)) * (DN_DK ** -0.5)
        d_k = l2_norm(d_k.reshape(b_, s_, DN_HEADS, DN_DK))
        d_v = d_v.reshape(b_, s_, DN_HEADS, DN_DV)
        beta = jax.nn.sigmoid(dn_b)
        g_log = -jnp.exp(a_log[l].astype(jnp.float32)) * jax.nn.softplus(
            dn_a.astype(jnp.float32) + dt_bias[l].astype(jnp.float32))
        o_a = gated_delta_rule(d_q, d_k, d_v, beta, g_log)
        o_a = rms_norm(o_a, dn_norm[l]) * jax.nn.silu(dn_z.reshape(b_, s_, DN_HEADS, DN_DV))
        y_a = o_a.reshape(b_, s_, DN_HEADS * DN_DV) @ w_oa[l]
        n_q = apply_rope(rms_norm(n_q.reshape(b_, s_, NSA_HEADS, dh), q_norm[l]), cos, sin)
        k_c = compress_blocks(c_k.reshape(b_, s_, gq, dh), cmp_pos[l, 0], w_cmp1[l, 0], w_cmp2[l, 0])
        k_c = apply_rope(rms_norm(k_c, k_norm[l, 0]), cos_c, sin_c)
        v_c = compress_blocks(c_v.reshape(b_, s_, gq, dh), cmp_pos[l, 1], w_cmp1[l, 1], w_cmp2[l, 1])
        s_k = apply_rope(rms_norm(s_k.reshape(b_, s_, gq, dh), k_norm[l, 1]), cos, sin)
        w_k = apply_rope(rms_norm(w_k.reshape(b_, s_, gq, dh), k_norm[l, 2]), cos, sin)
        gates = jax.nn.sigmoid(n_g.reshape(b_, s_, NSA_HEADS, 3))
        o_b = nsa_core(n_q, k_c, v_c, s_k, s_v.reshape(b_, s_, gq, dh),
                       w_k, w_v.reshape(b_, s_, gq, dh), gates)
        y_b = o_b @ w_ob[l]
        gate_a, gate_b = jnp.split(jax.nn.sigmoid(m_g), 2, -1)
        y = (gate_a * y_a + gate_b * y_b) @ w_out[l]
        x = x + g_m * y
        h = rms_norm(x, norm_ffn[l]) * (1.0 + sc_f) + sh_f
        if l % 2 == 0:
            f = swiglu(h, w1_dense[l // 2], w3_dense[l // 2], w2_dense[l // 2])
        else:
            f = moe_ffn(h, w_router[l // 2], w1_moe[l // 2], w3_moe[l // 2], w2_moe[l // 2])
        x = x + g_f * f
    return x
```

```python
import contextlib
import numpy as np
import concourse.bass as bass
import concourse.mybir as mybir

F32 = mybir.dt.float32
BF16 = mybir.dt.bfloat16
I32 = mybir.dt.int32
ALU = mybir.AluOpType
AF = mybir.ActivationFunctionType
AX = mybir.AxisListType

NRING = 24


class V:
    __slots__ = ("ap", "key")

    def __init__(self, ap, key):
        self.ap = ap
        self.key = key


def _ap(x):
    return x.ap if isinstance(x, V) else x


class Prog:
    def __init__(self, nc, es):
        self.nc = nc
        self.es = es
        self.eng = {"pe": nc.tensor, "act": nc.scalar, "dve": nc.vector,
                    "pool": nc.gpsimd, "sp": nc.sync}
        self.sem = {e: es.enter_context(nc.semaphore("s_" + e)) for e in self.eng}
        self.cnt = {e: 0 for e in self.eng}
        self.pend = {e: False for e in self.eng}
        self.waited = {e: {} for e in self.eng}
        self.ring = [es.enter_context(nc.semaphore("r%d" % i)) for i in range(2 * NRING)]
        self.dma_n = [0, 0]
        self.state = {}
        self.uid = 0
        self.ninst = 0

    def sb(self, name, shape, dt=F32):
        self.uid += 1
        name = "%s_u%d" % (name, self.uid)
        t = self.es.enter_context(self.nc.sbuf_tensor(name, list(shape), dt))
        return t

    def ps(self, name, shape, dt=F32):
        t = self.es.enter_context(self.nc.psum_tensor(name, list(shape), dt))
        return t

    def dram(self, name, shape, dt=F32, kind="Internal"):
        return self.nc.dram_tensor(name, list(shape), dt, kind=kind)

    def key(self, base="k"):
        self.uid += 1
        return "%s%d" % (base, self.uid)

    def _st(self, k):
        s = self.state.get(k)
        if s is None:
            s = {"w": {}, "wd": [], "r": {}, "rd": []}
            self.state[k] = s
        return s

    def _wait(self, e, tok):
        eng = self.eng[e]
        if tok[0] == "c":
            _, e2, idx = tok
            if self.waited[e].get(e2, 0) >= idx:
                return
            if e2 == e and (e == "pe" or idx > self.cnt[e2]):
                return
            if idx > self.cnt[e2]:
                raise RuntimeError("wait on un-emitted inc: %s waits %s idx %d cnt %d" % (e, e2, idx, self.cnt[e2]))
            eng.wait_ge(self.sem[e2], idx)
            self.waited[e][e2] = idx
        else:
            _, slot, val = tok
            kk = ("d", slot)
            if self.waited[e].get(kk, 0) >= val:
                return
            eng.wait_ge(self.ring[slot], val)
            self.waited[e][kk] = val

    def _deps(self, e, reads, writes):
        toks = []
        for v in reads:
            if isinstance(v, V) and v.key is not None:
                s = self._st(v.key)
                toks += [("c", e2, i) for e2, i in s["w"].items()]
                toks += s["wd"]
        for v in writes:
            if isinstance(v, V) and v.key is not None:
                s = self._st(v.key)
                toks += [("c", e2, i) for e2, i in s["w"].items()]
                toks += s["wd"]
                toks += [("c", e2, i) for e2, i in s["r"].items()]
                toks += s["rd"]
        for t in toks:
            self._wait(e, t)

    def _record(self, tok, reads, writes):
        for v in reads:
            if isinstance(v, V) and v.key is not None:
                s = self._st(v.key)
                if tok[0] == "c":
                    s["r"][tok[1]] = tok[2]
                else:
                    s["rd"].append(tok)
        for v in writes:
            if isinstance(v, V) and v.key is not None:
                s = self._st(v.key)
                if tok[0] == "c":
                    s["w"] = {tok[1]: tok[2]}
                    s["wd"] = []
                else:
                    s["w"] = {}
                    s["wd"] = [tok]
                s["r"] = {}
                s["rd"] = []

    def op(self, e, fn, reads=(), writes=(), inc=True):
        self._deps(e, reads, writes)
        ins = fn(self.eng[e])
        self.ninst += 1
        if inc:
            self.cnt[e] += 1
            ins.then_inc(self.sem[e], 1)
            tok = ("c", e, self.cnt[e])
            self.pend[e] = False
        else:
            tok = ("c", e, self.cnt[e] + 1)
            self.pend[e] = True
        self._record(tok, reads, writes)
        return ins

    def dma(self, out, in_, q="sp", **kw):
        self._deps(q, [in_], [out])
        r = 1 if q == "pool" else 0
        slot = r * NRING + self.dma_n[r] % NRING
        val = 16 * (self.dma_n[r] // NRING + 1)
        if val > 16:
            self._wait(q, ("d", slot, val - 16))
        self.dma_n[r] += 1
        ins = self.eng[q].dma_start(out=_ap(out), in_=_ap(in_), **kw)
        ins.then_inc(self.ring[slot], 16)
        self.ninst += 1
        self._record(("d", slot, val), [in_], [out])
        return ins

    def barrier(self):
        for e in self.eng:
            if self.pend[e]:
                raise RuntimeError("pending un-inc'ed instruction on " + e)
        for e2 in self.eng:
            if e2 != "pool" and self.cnt[e2] > 0:
                self._wait("pool", ("c", e2, self.cnt[e2]))
        for r in range(2):
            lo = max(0, self.dma_n[r] - NRING)
            for i in range(lo, self.dma_n[r]):
                self._wait("pool", ("d", r * NRING + i % NRING, 16 * (i // NRING + 1)))
        self.op("pool", lambda g: g.memset(self._bar_t[:, :], 0.0), writes=[V(self._bar_t[:, :], "bar_t")])
        tok = ("c", "pool", self.cnt["pool"])
        for e in self.eng:
            if e != "pool":
                self._wait(e, tok)
                for e2 in self.eng:
                    self.waited[e][e2] = max(self.waited[e].get(e2, 0), self.cnt[e2] if e2 != "pool" else self.cnt["pool"])
        for e2 in self.eng:
            if e2 != "pool":
                self.waited["pool"][e2] = self.cnt[e2]
        self.state = {"bar_t": {"w": {"pool": self.cnt["pool"]}, "wd": [], "r": {}, "rd": []}}

    def init(self):
        self._bar_t = self.sb("bar_t", [128, 8], F32)

    def mm(self, out, lhsT, rhs, start=True, stop=True, inc=None):
        if inc is None:
            inc = stop
        return self.op("pe", lambda t: t.matmul(_ap(out), _ap(lhsT), _ap(rhs), start=start, stop=stop),
                       reads=[lhsT, rhs], writes=[out], inc=inc)

    def transpose(self, out, in_, ident, inc=True):
        return self.op("pe", lambda t: t.transpose(_ap(out), _ap(in_), _ap(ident)),
                       reads=[in_, ident], writes=[out], inc=inc)

    def act(self, out, in_, func, bias=None, scale=None, accum=None, e="act"):
        kw = {}
        rd = [in_]
        wr = [out]
        if bias is not None:
            kw["bias"] = _ap(bias)
            rd.append(bias)
        if scale is not None:
            kw["scale"] = _ap(scale)
            rd.append(scale)
        if accum is not None:
            kw["accum_out"] = _ap(accum)
            wr.append(accum)
        return self.op(e, lambda a: a.activation(_ap(out), _ap(in_), func, **kw), reads=rd, writes=wr)

    def tt(self, out, a, b, op, e="dve"):
        return self.op(e, lambda v: v.tensor_tensor(_ap(out), _ap(a), _ap(b), op), reads=[a, b], writes=[out])

    def ts(self, out, a, s1, op0, s2=None, op1=None, accum=None, e="dve"):
        rd = [a, s1, s2]
        wr = [out]
        kw = {}
        if op1 is not None:
            kw["op1"] = op1
        if accum is not None:
            kw["accum_out"] = _ap(accum)
            wr.append(accum)
        return self.op(e, lambda v: v.tensor_scalar(_ap(out), _ap(a), _ap(s1), _ap(s2), op0, **kw), reads=rd, writes=wr)

    def stt(self, out, a, s, b, op0, op1, e="dve"):
        return self.op(e, lambda v: v.scalar_tensor_tensor(_ap(out), _ap(a), _ap(s), _ap(b), op0, op1),
                       reads=[a, s, b], writes=[out])

    def copy(self, out, in_, e="dve"):
        if e == "act":
            return self.op(e, lambda a: a.copy(_ap(out), _ap(in_)), reads=[in_], writes=[out])
        return self.op(e, lambda v: v.tensor_copy(_ap(out), _ap(in_)), reads=[in_], writes=[out])

    def memset(self, out, val, e="dve"):
        return self.op(e, lambda v: v.memset(_ap(out), val), reads=[], writes=[out])

    def reduce(self, out, in_, op, axis=AX.X, e="dve"):
        return self.op(e, lambda v: v.tensor_reduce(_ap(out), _ap(in_), axis, op), reads=[in_], writes=[out])

    def finish(self, out_tokens_engine="sp"):
        self.barrier()


from concourse.bass_utils import run_bass_kernel_spmd

D = 1024
PIN = 5416
FFN = 3584
NEXP = 8
O_QKV, O_Z, O_B, O_A, O_NQ, O_CK, O_CV, O_SK, O_SV, O_WK, O_WV, O_NG, O_MG = (
    0, 1536, 2048, 2056, 2064, 2576, 2704, 2832, 2960, 3088, 3216, 3344, 3368)
EPS = 1e-6
TWO_PI = 2.0 * np.pi


class Ctx:
    pass


class Pool:
    def __init__(self, P, name, n, shape, dt=F32):
        self.t = [P.sb("%s_%d" % (name, i), shape, dt) for i in range(n)]
        self.k = ["%s_%d" % (name, i) for i in range(n)]
        self.i = 0

    def next(self):
        j = self.i % len(self.t)
        self.i += 1
        return self.t[j], self.k[j]


def rsqrt_(P, out, in_, scale, bias_t):
    P.act(out, in_, AF.Sqrt, bias=bias_t, scale=scale)
    P.op("dve", lambda v: v.reciprocal(_ap(out), _ap(out)), reads=[out], writes=[out])


def stage0(P, C):
    nc = P.nc
    L, S = C.L, C.S
    with contextlib.ExitStack() as es:
        P.es, old = es, P.es
        cT = P.sb("s0_cT", [128, 8])
        cs = P.sb("s0_cs", [128, 8])
        wch = [P.sb("s0_w%d" % i, [128, 8, 512]) for i in range(2)]
        brow = P.sb("s0_b", [1, 6144])
        orow = P.sb("s0_o", [1, 6144])
        P.dma(V(cT[:, :], "cT"), C.c.rearrange("(c p) -> p c", p=128))
        P.act(V(cs[:, :], "cs"), V(cT[:, :], "cT"), AF.Silu)
        for l in range(L):
            P.dma(V(brow[:, :], "brow"), C.b_ada[l:l + 1, :])
            for j in range(12):
                w = wch[j % 2]
                wk = "s0w%d" % (j % 2)
                P.dma(V(w[:, :, :], wk), C.w_ada[l, :, j * 512:(j + 1) * 512].rearrange("(c p) n -> p c n", p=128),
                      q=("sp" if j % 2 == 0 else "act"))
                pb = C.psb[j % 2]
                pk = "psb%d" % (j % 2)
                for k in range(8):
                    P.mm(V(pb[0:1, :], pk), V(cs[:, k:k + 1], "cs"), V(w[:, k, :], wk), start=(k == 0), stop=(k == 7))
                P.tt(V(orow[:, j * 512:(j + 1) * 512], "orow"), V(pb[0:1, :], pk), V(brow[:, j * 512:(j + 1) * 512], "brow"), ALU.add)
            P.dma(V(C.mod_d[l:l + 1, :], "mod_d"), V(orow[:, :], "orow"))
        NT = S // 128
        posi = P.sb("s0_posi", [128, NT], I32)
        posf = P.sb("s0_posf", [128, NT])
        invf = P.sb("s0_invf", [128, 32])
        P.dma(V(posi[:, :], "posi"), C.pos.rearrange("(n p) -> p n", p=128))
        P.dma(V(invf[:, :], "invf"), C.invf)
        P.copy(V(posf[:, :], "posf"), V(posi[:, :], "posi"))
        up = Pool(P, "s0_u", 2, [128, 64])
        uip = Pool(P, "s0_ui", 2, [128, 64], I32)
        ufp = Pool(P, "s0_uf", 2, [128, 64])
        obp = Pool(P, "s0_ob", 2, [128, 64])
        for tt in range(NT):
            u, ku = up.next()
            ui, kui = uip.next()
            uf, kuf = ufp.next()
            ob, kob = obp.next()
            P.ts(V(u[:, 32:64], ku), V(invf[:, :], "invf"), V(posf[:, tt:tt + 1], "posf"), ALU.mult)
            P.ts(V(u[:, 0:32], ku), V(u[:, 32:64], ku), 0.25, ALU.add)
            P.copy(V(ui[:, :], kui), V(u[:, :], ku))
            P.copy(V(uf[:, :], kuf), V(ui[:, :], kui))
            P.tt(V(u[:, :], ku), V(u[:, :], ku), V(uf[:, :], kuf), ALU.subtract)
            P.act(V(ob[:, :], kob), V(u[:, :], ku), AF.Sin, scale=TWO_PI)
            P.dma(V(C.cs_d[tt * 128:(tt + 1) * 128, :], "cs_d%d" % tt), V(ob[:, :], kob))
        P.barrier()
        P.es = old


def load_layer_vecs(P, C, l, which):
    i0 = 0 if which == "m" else 3
    nwd = C.norm_mix if which == "m" else C.norm_ffn
    nw = P.sb("lv_nw", [128, 8])
    nsc = P.sb("lv_nsc", [128, 8])
    nsh = P.sb("lv_nsh", [128, 8])
    grow = P.sb("lv_g", [128, D])
    P.dma(V(nw[:, :], "lv_nw"), nwd[l, :].rearrange("(c p) -> p c", p=128))
    P.dma(V(nsh[:, :], "lv_nsh"), V(C.mod_d[l, (i0 + 0) * D:(i0 + 1) * D].rearrange("(c p) -> p c", p=128), "mod_d"))
    P.dma(V(nsc[:, :], "lv_nsc"), V(C.mod_d[l, (i0 + 1) * D:(i0 + 2) * D].rearrange("(c p) -> p c", p=128), "mod_d"))
    P.dma(V(grow[:, :], "lv_g"), V(C.mod_d[l:l + 1, (i0 + 2) * D:(i0 + 3) * D].partition_broadcast(128), "mod_d"))
    P.stt(V(nsc[:, :], "lv_nsc"), V(nsc[:, :], "lv_nsc"), 1.0, V(nw[:, :], "lv_nw"), ALU.add, ALU.mult)
    return V(nsc[:, :], "lv_nsc"), V(nsh[:, :], "lv_nsh"), V(grow[:, :], "lv_g")


def norm_mod_T(P, C, xt, kx, nsc, nsh, hT, khT, tok0=0, hT32=None):
    ss, kss = C.p_ss.next()
    sq, ksq = C.p_sq.next()
    P.act(V(sq[:, :], ksq), V(xt[:, :], kx), AF.Square, accum=V(ss[:, :], kss))
    rsqrt_(P, V(ss[:, :], kss), V(ss[:, :], kss), 1.0 / D, C.eps_t)
    P.ts(V(sq[:, :], ksq), V(xt[:, :], kx), V(ss[:, 0:1], kss), ALU.mult)
    for half in range(2):
        pb, pk = C.psum()
        for c4 in range(4):
            c = half * 4 + c4
            P.transpose(V(pb[:, c4 * 128:(c4 + 1) * 128], pk), V(sq[:, c * 128:(c + 1) * 128], ksq), C.ident, inc=(c4 == 3))
        for c4 in range(4):
            c = half * 4 + c4
            P.act(V(hT[:, c, tok0:tok0 + 128], khT), V(pb[:, c4 * 128:(c4 + 1) * 128], pk), AF.Identity,
                  bias=V(_ap(nsh)[:, c:c + 1], nsh.key), scale=V(_ap(nsc)[:, c:c + 1], nsc.key))
            if hT32 is not None:
                P.act(V(hT32[0][:, c, :], hT32[1]), V(pb[:, c4 * 128:(c4 + 1) * 128], pk), AF.Identity,
                      bias=V(_ap(nsh)[:, c:c + 1], nsh.key), scale=V(_ap(nsc)[:, c:c + 1], nsc.key))


def stage1(P, C, l):
    S = C.S
    with contextlib.ExitStack() as es:
        P.es, old = es, P.es
        nsc, nsh, grow = load_layer_vecs(P, C, l, "m")
        W = P.sb("s1_W", [128, 8, PIN], BF16)
        ncol = [(j * 512, min(512, PIN - j * 512)) for j in range((PIN + 511) // 512)]
        for (c0, cw) in ncol:
            P.dma(V(W[:, :, c0:c0 + cw], "s1W%d" % c0), C.w_in[l, :, c0:c0 + cw].rearrange("(c p) n -> p c n", p=128), q="pool")
        xp = Pool(P, "s1_x", 2, [128, D])
        hp = Pool(P, "s1_h", 2, [128, 8, 128], BF16)
        op_ = Pool(P, "s1_o", 2, [128, PIN])
        C.p_ss = Pool(P, "s1_ss", 2, [128, 1])
        C.p_sq = Pool(P, "s1_sq", 2, [128, D])
        for tt in range(S // 128):
            xt, kx = xp.next()
            hT, khT = hp.next()
            ot, ko = op_.next()
            P.dma(V(xt[:, :], kx), V(C.xcur[tt * 128:(tt + 1) * 128, :], "xcur%d" % tt))
            norm_mod_T(P, C, xt, kx, nsc, nsh, hT, khT)
            for j, (c0, cw) in enumerate(ncol):
                pb, pk = C.psum()
                for k in range(8):
                    P.mm(V(pb[:, 0:cw], pk), V(hT[:, k, :], khT), V(W[:, k, c0:c0 + cw], "s1W%d" % c0), start=(k == 0), stop=(k == 7))
                P.copy(V(ot[:, c0:c0 + cw], ko), V(pb[:, 0:cw], pk), e=("act" if j % 2 == 0 else "dve"))
            P.dma(V(C.proj_d[tt * 128:(tt + 1) * 128, :], "proj%d" % tt), V(ot[:, :], ko), q="act")
        P.barrier()
        P.es = old


def build_program(S, L, dbg=()):
    nc = bass.Bass("TRN2", target_bir_lowering=False)
    C = Ctx()
    C.S, C.L = S, L
    import os
    C.cut = int(os.environ.get('CUT', '0'))
    C.skip = os.environ.get('SKIP', '').split(',')
    din = lambda name, shape, dt=F32: nc.dram_tensor(name, list(shape), dt, kind="ExternalInput").ap()
    C.x = din("x", [S, D])
    C.c = din("c", [D])
    C.pos = din("positions", [S], I32)
    C.w_ada = din("w_ada", [L, D, 6 * D])
    C.b_ada = din("b_ada", [L, 6 * D])
    C.norm_mix = din("norm_mix", [L, D])
    C.norm_ffn = din("norm_ffn", [L, D])
    C.w_in = din("w_in", [L, D, PIN])
    C.conv_w = din("conv_w", [L, 4, 1536])
    C.a_log = din("a_log", [L, 8])
    C.dt_bias = din("dt_bias", [L, 8])
    C.dn_norm = din("dn_norm", [L, 64])
    C.cmp_pos = din("cmp_pos", [L, 2, 32, 64])
    C.w_cmp1 = din("w_cmp1", [L, 2, 2048, 64])
    C.w_cmp2 = din("w_cmp2", [L, 2, 64, 64])
    C.q_norm = din("q_norm", [L, 64])
    C.k_norm = din("k_norm", [L, 3, 64])
    C.w_oa = din("w_oa", [L, 512, D])
    C.w_ob = din("w_ob", [L, 512, D])
    C.w_out = din("w_out", [L, D, D])
    nd, nm = (L + 1) // 2, max(L // 2, 1)
    C.w1_dense = din("w1_dense", [nd, D, FFN])
    C.w3_dense = din("w3_dense", [nd, D, FFN])
    C.w2_dense = din("w2_dense", [nd, FFN, D])
    C.w_router = din("w_router", [nm, D, NEXP])
    C.w1_moe = din("w1_moe", [nm, NEXP, D, FFN])
    C.w3_moe = din("w3_moe", [nm, NEXP, D, FFN])
    C.w2_moe = din("w2_moe", [nm, NEXP, FFN, D])
    C.ident_d = din("k_ident", [128, 128])
    C.invf = din("k_invf", [128, 32])
    C.k_U = din("k_U", [128, 128])
    C.k_NM1s = din("k_NM1s", [128, 128])
    C.k_M2 = din("k_M2", [128, 128])
    C.k_SU = din("k_SU", [128, 128])
    NTc = S // 128
    C.k_Ov = din("k_Ov", [128, 4, 128])
    C.k_Ex = din("k_Ex", [128, NTc, 128])
    C.k_Mc = din("k_Mc", [128, 2304 + 128])
    C.k_Km = din("k_Km", [128, 256])
    C.k_Fm = din("k_Fm", [128, 256])
    C.out = nc.dram_tensor("out", [S, D], F32, kind="ExternalOutput").ap()
    dk = lambda name: ("ExternalOutput" if name in dbg else "Internal")
    dscr = lambda name, shape, dt=F32: nc.dram_tensor(name, list(shape), dt, kind=dk(name)).ap()
    C.mod_d = dscr("mod_d", [L, 6 * D])
    C.cs_d = dscr("cs_d", [S, 64])
    C.proj_d = dscr("proj_d", [S, PIN])
    C.oa_d = dscr("oa_d", [S, 512])
    C.ob_d = dscr("ob_d", [S, 512])
    C.xmid_d = dscr("xmid_d", [S, D])
    C.xl_d = dscr("xl_d", [S, D])
    C.qT_d = dscr("qT_d", [8, 64, S], BF16)
    C.kT_d = dscr("kT_d", [4, 64, S], BF16)
    C.xcur = C.x
    with contextlib.ExitStack() as es:
        es.enter_context(nc.allow_non_contiguous_dma(reason="small strided parameter loads"))
        P = Prog(nc, es)
        P.init()
        C.P = P
        C.psb = [P.ps("psb%d" % i, [128, 512]) for i in range(8)]
        C.psi = 0

        C.psn = 8

        def psum():
            j = C.psi % C.psn
            C.psi += 1
            return C.psb[j], "psb%d" % j
        C.psum = psum
        ident = P.sb("ident", [128, 128])
        eps_t = P.sb("eps_t", [128, 1])
        P.dma(V(ident[:, :], "ident"), C.ident_d)
        P.memset(V(eps_t[:, :], "eps"), EPS)
        one_t = P.sb("one_t", [128, 1])
        P.memset(V(one_t[:, :], "one"), 1.0)
        C.one_t = one_t[:, :]
        P.barrier()
        C.ident = ident[:, :]
        C.eps_t = eps_t[:, :]
        stage0(P, C)
        for l in range(L):
            C.xcur = C.x if l == 0 else C.xl_d
            stage1(P, C, l)
            if "s2" not in C.skip:
                stage2(P, C, l)
            if "s3" not in C.skip:
                stage3(P, C, l)
            if "s4" not in C.skip:
                stage4(P, C, l)
                stage5(P, C, l, C.out if l == L - 1 else C.xl_d)
        P.finish()
        print("ninst", P.ninst)
    return nc


def host_consts(S=8192):
    NTc = S // 128
    cc = np.arange(128)[:, None]
    jj = np.arange(128)[None, :]
    Ov = np.zeros((128, 4, 128), np.float32)
    for ct in range(4):
        cg = ct * 128 + cc
        Ov[:, ct, :] = ((16 * cg < 64 * jj + 64) & (16 * cg + 32 > 64 * jj))
    Ex = np.zeros((128, NTc, 128), np.float32)
    for kt in range(NTc):
        Ex[:, kt, :] = (cc == 2 * kt + jj // 64)
    col = np.arange(2304 + 128)[None, :]
    Mc = (col >= 16 * cc + 31).astype(np.float32)
    r = np.arange(128)[:, None]
    jp = np.arange(256)[None, :] - 128
    cur = r // 64
    Km = (jp < cur - 1).astype(np.float32)
    Fm = np.where(jp > cur, -FORCE, np.where(jp == cur, FORCE, np.where(jp == cur - 1, FORCE + 64.0, 0.0))).astype(np.float32)
    nsa = {"k_Ov": Ov, "k_Ex": Ex, "k_Mc": Mc, "k_Km": np.ascontiguousarray(Km), "k_Fm": np.ascontiguousarray(Fm)}
    d = _host_consts0()
    d.update(nsa)
    return d


def _host_consts0():
    inv = 1.0 / (10000.0 ** (np.arange(0, 64, 2, dtype=np.float32) / 64.0))
    inv = (inv.astype(np.float64) / (2 * np.pi)).astype(np.float32)
    p = np.arange(128)[:, None]
    j = np.arange(128)[None, :]
    f = lambda m: np.ascontiguousarray(m.astype(np.float32))
    return {"k_U": f(p <= j), "k_NM1s": f(np.where(j >= p, -BIG, 0.0)), "k_M2": f(np.where(j < p, -BIG, 0.0)),
            "k_SU": f(j > p),
            "k_ident": np.eye(128, dtype=np.float32),
            "k_invf": np.ascontiguousarray(np.broadcast_to(inv[None, :], (128, 32))).astype(np.float32)}


BIG = 30000.0


def stage2(P, C, l):
    S = C.S
    NCH = S // 128
    with contextlib.ExitStack() as es:
        P.es, old = es, P.es
        sb = P.sb
        ident = C.ident
        cw = sb("d_cw", [128, 4, 12])
        negA = sb("d_negA", [128, 8])
        dtb = sb("d_dtb", [128, 8])
        dnw = sb("d_dnw", [128, 64])
        Um = sb("d_U", [128, 128])
        NM1s = sb("d_NM1s", [128, 128])
        M2 = sb("d_M2", [128, 128])
        SU = sb("d_SU", [128, 128])
        ones128 = sb("d_ones", [128, 128])
        P.memset(V(ones128[:, :], "ones128"), 1.0)
        for k in range(4):
            P.dma(V(cw[:, k, :], "cw"), C.conv_w[l, k, :].rearrange("(c p) -> p c", p=128))
        P.dma(V(negA[:, :], "negA"), C.a_log[l:l + 1, :].partition_broadcast(128))
        P.dma(V(dtb[:, :], "dtb"), C.dt_bias[l:l + 1, :].partition_broadcast(128))
        P.dma(V(dnw[:, :], "dnw"), C.dn_norm[l:l + 1, :].partition_broadcast(128))
        P.dma(V(Um[:, :], "U"), C.k_U)
        P.dma(V(NM1s[:, :], "NM1s"), C.k_NM1s)
        P.dma(V(M2[:, :], "M2"), C.k_M2)
        P.dma(V(SU[:, :], "SU"), C.k_SU)
        P.act(V(negA[:, :], "negA"), V(negA[:, :], "negA"), AF.Exp)
        P.ts(V(negA[:, :], "negA"), V(negA[:, :], "negA"), -1.0, ALU.mult)
        fT = sb("d_fT", [128, 12, 131])
        P.memset(V(fT[:, :, :], "fT"), 0.0)
        Sst = sb("d_S", [64, 8, 64])
        P.memset(V(Sst[:, :, :], "S"), 0.0)
        pt = sb("d_pt", [128, 2064])
        cv = sb("d_cv", [128, 12, 128])
        qkv = sb("d_qkv", [128, 1536])
        sq = sb("d_sq", [128, 1024])
        ssn = sb("d_ssn", [128, 16])
        qkT = sb("d_qkT", [64, 16, 128])
        sc8 = {n: sb("d_" + n, [128, 8]) for n in ("g", "beta", "nbeta", "gc", "ngc", "egc", "bege", "kdf", "gl", "t8")}
        grep_ = [sb("d_grep%d" % h, [128, 128]) for h in range(8)]
        tmp1 = [sb("d_tmp1_%d" % h, [128, 128]) for h in range(8)]
        tmp2 = [sb("d_tmp2_%d" % h, [128, 128]) for h in range(8)]
        decS = [sb("d_decS%d" % h, [128, 128]) for h in range(8)]
        decT = [sb("d_decT%d" % h, [128, 128]) for h in range(8)]
        AT = [sb("d_AT%d" % h, [128, 128]) for h in range(8)]
        Pm = [[sb("d_P%d_%d" % (h, i), [128, 128]) for i in range(2)] for h in range(8)]
        PTm = [[sb("d_PT%d_%d" % (h, i), [128, 128]) for i in range(2)] for h in range(8)]
        XT = [[sb("d_XT%d_%d" % (h, i), [128, 128]) for i in range(2)] for h in range(8)]
        vb = sb("d_vb", [128, 512])
        kbe = sb("d_kbe", [128, 512])
        kdec = sb("d_kdec", [128, 512])
        u = sb("d_u", [128, 512])
        wT = sb("d_wT", [64, 8, 128])
        vnew = sb("d_vnew", [128, 512])
        tq = sb("d_tq", [128, 512])
        o = sb("d_o", [128, 512])
        osq = sb("d_osq", [128, 512])
        zs = sb("d_zs", [128, 512])
        oo = sb("d_oo", [128, 512])
        s8 = lambda n: V(sc8[n][:, :], "sc_" + n)
        for n in range(NCH):
            P.dma(V(pt[:, :], "pt"), V(C.proj_d[n * 128:(n + 1) * 128, 0:2064], "proj%d" % n))
            P.copy(V(fT[:, :, 0:3], "fT"), V(fT[:, :, 128:131], "fT"), e="pool")
            for b in range(3):
                pb, pk = C.psum()
                for c4 in range(4):
                    c = b * 4 + c4
                    P.transpose(V(pb[:, c4 * 128:(c4 + 1) * 128], pk), V(pt[:, c * 128:(c + 1) * 128], "pt"), ident, inc=(c4 == 3))
                P.copy(V(fT[:, b * 4:(b + 1) * 4, 3:131], "fT"), V(pb[:, :].rearrange("p (c t) -> p c t", c=4), pk),
                       e=("act" if b % 2 == 0 else "dve"))
            for c in range(12):
                b = c // 4
                e = "dve"
                P.ts(V(cv[:, c, :], "cvg%d" % b), V(fT[:, c, 0:128], "fT"), V(cw[:, 0, c:c + 1], "cw"), ALU.mult, e=e)
                for k in range(1, 4):
                    P.stt(V(cv[:, c, :], "cvg%d" % b), V(fT[:, c, k:k + 128], "fT"), V(cw[:, k, c:c + 1], "cw"),
                          V(cv[:, c, :], "cvg%d" % b), ALU.mult, ALU.add, e=e)
            for b in range(3):
                P.act(V(cv[:, b * 4:(b + 1) * 4, :], "cvg%d" % b), V(cv[:, b * 4:(b + 1) * 4, :], "cvg%d" % b), AF.Silu)
            for b in range(3):
                pb, pk = C.psum()
                for c4 in range(4):
                    c = b * 4 + c4
                    P.transpose(V(pb[:, c4 * 128:(c4 + 1) * 128], pk), V(cv[:, c, :], "cvg%d" % b), ident, inc=(c4 == 3))
                P.copy(V(qkv[:, b * 512:(b + 1) * 512], "qkv"), V(pb[:, :], pk), e=("act" if b % 2 == 1 else "dve"))
            if C.cut == 1:
                continue
            P.tt(V(sq[:, :], "sq"), V(qkv[:, 0:1024], "qkv"), V(qkv[:, 0:1024], "qkv"), ALU.mult)
            P.reduce(V(ssn[:, :], "ssn"), V(sq[:, :].rearrange("p (h d) -> p h d", d=64), "sq"), ALU.add)
            rsqrt_(P, V(ssn[:, :], "ssn"), V(ssn[:, :], "ssn"), 1.0, C.eps_t)
            P.ts(V(ssn[:, 0:8], "ssn"), V(ssn[:, 0:8], "ssn"), 0.125, ALU.mult)
            P.tt(V(qkv[:, 0:1024].rearrange("p (h d) -> p h d", d=64), "qkv"),
                 V(qkv[:, 0:1024].rearrange("p (h d) -> p h d", d=64), "qkv"),
                 V(ssn[:, :].unsqueeze(2).to_broadcast([128, 16, 64]), "ssn"), ALU.mult)
            if C.cut == 11:
                continue
            for b in range(4):
                pb, pk = C.psum()
                for c4 in range(4):
                    hh = b * 4 + c4
                    P.mm(V(pb[0:64, c4 * 128:(c4 + 1) * 128], pk), V(qkv[:, hh * 64:(hh + 1) * 64], "qkv"), ident, inc=(c4 == 3))
                P.copy(V(qkT[:, b * 4:(b + 1) * 4, :], "qkT"), V(pb[0:64, :].rearrange("p (c t) -> p c t", c=4), pk),
                       e=("act" if b % 2 == 0 else "dve"))
            if C.cut == 2:
                continue
            P.tt(s8("t8"), V(pt[:, O_A:O_A + 8], "pt"), V(dtb[:, :], "dtb"), ALU.add)
            P.act(s8("t8"), s8("t8"), AF.Exp)
            P.act(s8("t8"), s8("t8"), AF.Ln, bias=C.one_t)
            if C.cut == 31:
                continue
            P.tt(s8("g"), s8("t8"), V(negA[:, :], "negA"), ALU.mult)
            P.act(s8("beta"), V(pt[:, O_B:O_B + 8], "pt"), AF.Sigmoid)
            P.ts(s8("nbeta"), s8("beta"), -1.0, ALU.mult)
            if C.cut == 32:
                continue
            pb, pk = C.psum()
            P.mm(V(pb[:, 0:8], pk), V(Um[:, :], "U"), s8("g"))
            P.copy(s8("gc"), V(pb[:, 0:8], pk))
            if C.cut == 33:
                continue
            P.ts(s8("ngc"), s8("gc"), -1.0, ALU.mult)
            P.act(s8("egc"), s8("gc"), AF.Exp)
            P.tt(s8("bege"), s8("beta"), s8("egc"), ALU.mult)
            if C.cut == 3:
                continue
            for h in range(8):
                kh = "h%d" % h
                P.ts(V(grep_[h][:, :], "grep" + kh), V(ones128[:, :], "ones128"), V(sc8["g"][:, h:h + 1], "sc_g"), ALU.mult)
                if 41 <= C.cut <= 41:
                    continue
                pg, pgk = C.psum()
                P.mm(V(pg[:, 0:128], pgk), V(grep_[h][:, :], "grep" + kh), V(Um[:, :], "U"))
                if 41 <= C.cut <= 42:
                    continue
                P.stt(V(tmp1[h][:, :], "tmp1" + kh), V(pg[:, 0:128], pgk), -1.0, V(NM1s[:, :], "NM1s"), ALU.mult, ALU.add)
                P.tt(V(tmp2[h][:, :], "tmp2" + kh), V(pg[:, 0:128], pgk), V(M2[:, :], "M2"), ALU.add)
                if 41 <= C.cut <= 43:
                    continue
                P.act(V(sc8["gl"][:, h:h + 1], "sc_gl"), V(tmp2[h][:, 127:128], "tmp2" + kh), AF.Exp)
                if 41 <= C.cut <= 44:
                    continue
                P.act(V(decS[h][:, :], "decS" + kh), V(tmp1[h][:, :], "tmp1" + kh), AF.Exp, bias=V(sc8["gc"][:, h:h + 1], "sc_gc"))
                P.act(V(decT[h][:, :], "decT" + kh), V(tmp2[h][:, :], "tmp2" + kh), AF.Exp, bias=V(sc8["ngc"][:, h:h + 1], "sc_ngc"))
                if 41 <= C.cut <= 45:
                    continue
                P.copy(V(sc8["kdf"][:, h:h + 1], "sc_kdf"), V(decT[h][:, 127:128], "decT" + kh))
                if 41 <= C.cut <= 46:
                    continue
                pm, pmk = C.psum()
                P.mm(V(pm[:, 0:128], pmk), V(qkT[:, 8 + h, :], "qkT"), V(qkT[:, 8 + h, :], "qkT"), inc=False)
                P.mm(V(pm[:, 128:256], pmk), V(qkT[:, 8 + h, :], "qkT"), V(qkT[:, h, :], "qkT"))
                if 41 <= C.cut <= 47:
                    continue
                P.stt(V(Pm[h][0][:, :], "P0" + kh), V(pm[:, 0:128], pmk), V(sc8["nbeta"][:, h:h + 1], "sc_nbeta"),
                      V(decS[h][:, :], "decS" + kh), ALU.mult, ALU.mult)
                if 41 <= C.cut <= 48:
                    continue
                P.tt(V(AT[h][:, :], "AT" + kh), V(pm[:, 128:256], pmk), V(decT[h][:, :], "decT" + kh), ALU.mult)
                if 41 <= C.cut <= 49:
                    continue
                P.mm(V(pm[:, 256:384], pmk), V(Pm[h][0][:, :], "P0" + kh), ident)
                P.copy(V(PTm[h][0][:, :], "PT0" + kh), V(pm[:, 256:384], pmk), e="dve")
                P.tt(V(XT[h][0][:, :], "XT0" + kh), V(pm[:, 256:384], pmk), ident, ALU.add)
            if C.cut == 4 or C.cut >= 40:
                continue
            q3 = lambda t: t[:, :].rearrange("p (h d) -> p h d", d=64)
            bc = lambda n: V(sc8[n][:, :].unsqueeze(2).to_broadcast([128, 8, 64]), "sc_" + n)
            P.tt(V(q3(vb), "vb"), V(qkv[:, 1024:1536].rearrange("p (h d) -> p h d", d=64), "qkv"), bc("beta"), ALU.mult)
            P.tt(V(q3(kbe), "kbe"), V(qkv[:, 512:1024].rearrange("p (h d) -> p h d", d=64), "qkv"), bc("bege"), ALU.mult)
            P.tt(V(q3(kdec), "kdec"), V(qkv[:, 512:1024].rearrange("p (h d) -> p h d", d=64), "qkv"), bc("kdf"), ALU.mult)
            if C.cut == 5:
                continue
            for lev in range(6):
                a, b2 = lev % 2, (lev + 1) % 2
                for h in range(8):
                    kh = "h%d" % h
                    kP, kPT, kX = "P%d" % a + kh, "PT%d" % a + kh, "XT%d" % a + kh
                    nP, nPT, nX = "P%d" % b2 + kh, "PT%d" % b2 + kh, "XT%d" % b2 + kh
                    pm, pmk = C.psum()
                    P.mm(V(pm[:, 0:128], pmk), V(PTm[h][a][:, :], kPT), V(Pm[h][a][:, :], kP), inc=(lev == 5))
                    if lev < 5:
                        P.mm(V(pm[:, 128:256], pmk), V(Pm[h][a][:, :], kP), V(PTm[h][a][:, :], kPT))
                    P.copy(V(Pm[h][b2][:, :], nP), V(pm[:, 0:128], pmk), e="act")
                    if lev < 5:
                        P.copy(V(PTm[h][b2][:, :], nPT), V(pm[:, 128:256], pmk), e="act")
                    px, pxk = C.psum()
                    P.mm(V(px[:, 0:128], pxk), V(Pm[h][b2][:, :], nP), V(XT[h][a][:, :], kX))
                    P.tt(V(XT[h][b2][:, :], nX), V(px[:, 0:128], pxk), V(XT[h][a][:, :], kX), ALU.add)
            if C.cut == 6:
                continue
            pu, puk = C.psum()
            pw, pwk = C.psum()
            for h in range(8):
                kX = "XT0h%d" % h
                P.mm(V(pu[:, h * 64:(h + 1) * 64], puk), V(XT[h][0][:, :], kX), V(vb[:, h * 64:(h + 1) * 64], "vb"), inc=(h == 7))
            for h in range(8):
                kX = "XT0h%d" % h
                if h == 4:
                    pw2, pw2k = C.psum()
                pwb, pwbk = (pw, pwk) if h < 4 else (pw2, pw2k)
                P.mm(V(pwb[0:64, (h % 4) * 128:(h % 4 + 1) * 128], pwbk), V(kbe[:, h * 64:(h + 1) * 64], "kbe"), V(XT[h][0][:, :], kX), inc=(h % 4 == 3))
            P.copy(V(u[:, :], "u"), V(pu[:, :], puk), e="act")
            P.copy(V(wT[:, 0:4, :], "wT"), V(pw[0:64, :].rearrange("p (c t) -> p c t", c=4), pwk), e="dve")
            P.copy(V(wT[:, 4:8, :], "wT"), V(pw2[0:64, :].rearrange("p (c t) -> p c t", c=4), pw2k), e="act")
            if C.cut == 7:
                continue
            p1, p1k = C.psum()
            p2, p2k = C.psum()
            for h in range(8):
                P.mm(V(p1[:, h * 64:(h + 1) * 64], p1k), V(wT[:, h, :], "wT"), V(Sst[:, h, :], "S"), inc=(h == 7))
            for h in range(8):
                P.mm(V(p2[:, h * 64:(h + 1) * 64], p2k), V(qkT[:, h, :], "qkT"), V(Sst[:, h, :], "S"), inc=(h == 7))
            P.tt(V(vnew[:, :], "vnew"), V(u[:, :], "u"), V(p1[:, :], p1k), ALU.subtract)
            P.tt(V(q3(tq), "tq"), V(p2[:, :].rearrange("p (h d) -> p h d", d=64), p2k), bc("egc"), ALU.mult)
            p3, p3k = C.psum()
            p4, p4k = C.psum()
            for h in range(8):
                P.mm(V(p3[:, h * 64:(h + 1) * 64], p3k), V(AT[h][:, :], "ATh%d" % h), V(vnew[:, h * 64:(h + 1) * 64], "vnew"), inc=(h == 7))
            for h in range(8):
                P.mm(V(p4[0:64, h * 64:(h + 1) * 64], p4k), V(kdec[:, h * 64:(h + 1) * 64], "kdec"), V(vnew[:, h * 64:(h + 1) * 64], "vnew"), inc=(h == 7))
            P.tt(V(o[:, :], "o"), V(p3[:, :], p3k), V(tq[:, :], "tq"), ALU.add)
            P.tt(V(Sst[:, :, :], "S"), V(Sst[:, :, :], "S"), V(sc8["gl"][0:64, :].unsqueeze(2).to_broadcast([64, 8, 64]), "sc_gl"), ALU.mult)
            P.tt(V(Sst[:, :, :], "S"), V(Sst[:, :, :], "S"), V(p4[0:64, :].rearrange("p (h d) -> p h d", d=64), p4k), ALU.add)
            if C.cut == 8:
                continue
            P.tt(V(osq[:, :], "osq"), V(o[:, :], "o"), V(o[:, :], "o"), ALU.mult, e="pool")
            P.reduce(s8("t8"), V(q3(osq), "osq"), ALU.add)
            rsqrt_(P, s8("t8"), s8("t8"), 1.0 / 64, C.eps_t)
            P.tt(V(q3(o), "o"), V(q3(o), "o"), bc("t8"), ALU.mult)
            P.tt(V(q3(o), "o"), V(q3(o), "o"), V(dnw[:, :].unsqueeze(1).to_broadcast([128, 8, 64]), "dnw"), ALU.mult)
            P.act(V(zs[:, :], "zs"), V(pt[:, O_Z:O_Z + 512], "pt"), AF.Silu)
            P.tt(V(oo[:, :], "oo"), V(o[:, :], "o"), V(zs[:, :], "zs"), ALU.mult)
            P.dma(V(C.oa_d[n * 128:(n + 1) * 128, :], "oa%d" % n), V(oo[:, :], "oo"), q="act")
        P.barrier()
        P.es = old


FORCE = 1.0e6
O_N0 = O_NQ
N_NSA = O_MG - O_NQ


def stage3(P, C, l):
    S = C.S
    NT = S // 128
    NCMP = (S - 32) // 16 + 1
    NCT = (NCMP + 127) // 128
    ident = C.ident
    with contextlib.ExitStack() as es0:
        P.es, old = es0, P.es
        sb = P.sb
        identb = sb("n_identb", [128, 128], BF16)
        P.copy(V(identb[:, :], "identb"), ident)
        svp = sb("n_svp", [128, NT, 2, 65], BF16)
        wvp = sb("n_wvp", [128, NT, 2, 65], BF16)
        gates = sb("n_gates", [128, NT, 24])
        rhsc = sb("n_rhsc", [128, NCT, 2, 193], BF16)
        kcT = sb("n_kcT", [64, 2, NCT * 128], BF16)
        P.memset(V(svp[:, :, :, 64:65], "svp"), 1.0)
        P.memset(V(wvp[:, :, :, 64:65], "wvp"), 1.0)
        P.memset(V(rhsc[:, :, :, :], "rhsc"), 0.0)
        P.memset(V(kcT[:, :, :], "kcT"), 0.0)
        P.memset(V(rhsc[:, :, :, 64:65], "rhsc"), 1.0)
        ovt = sb("n_ovt", [128, NCT, 128])
        P.dma(V(ovt[:, :, :], "ovt"), C.k_Ov[:, 0:NCT, :])
        for g in range(2):
            P.copy(V(rhsc[:, :, g, 65:193], "rhsc"), V(ovt[:, :, :], "ovt"))
        with contextlib.ExitStack() as es1:
            P.es = es1
            ccT = sb("n_ccT", [64, 4, S], BF16)
            w20 = sb("n_w20", [128, 20, 64])
            P.memset(V(w20[:, :, :], "w20"), 1.0)
            for h in range(8):
                P.dma(V(w20[:, h, :], "w20"), C.q_norm[l:l + 1, :].partition_broadcast(128))
            for j, hh in ((1, 12), (1, 13), (2, 16), (2, 17)):
                P.dma(V(w20[:, hh, :], "w20"), C.k_norm[l, j:j + 1, :].partition_broadcast(128))
            P.ts(V(w20[:, 0:8, :], "w20"), V(w20[:, 0:8, :], "w20"), 0.125, ALU.mult)
            pt = sb("n_pt", [128, N_NSA])
            cst = sb("n_cs", [128, 64])
            sq = sb("n_sq", [128, 1280])
            ss = sb("n_ss", [128, 20])
            xn = sb("n_xn", [128, 20, 64])
            ro = sb("n_ro", [128, 20, 64])
            t1 = sb("n_t1", [128, 20, 32])
            rob = sb("n_rob", [128, 20, 64], BF16)
            ptb = sb("n_ptb", [128, 1280], BF16)
            qTt = sb("n_qTt", [64, 8, 128], BF16)
            kTt = sb("n_kTt", [64, 4, 128], BF16)
            for tt in range(NT):
                P.dma(V(pt[:, :], "pt"), V(C.proj_d[tt * 128:(tt + 1) * 128, O_N0:O_N0 + N_NSA], "proj%d" % tt))
                P.dma(V(cst[:, :], "cst"), V(C.cs_d[tt * 128:(tt + 1) * 128, :], "cs_d%d" % tt))
                x3 = pt[:, 0:1280].rearrange("p (h d) -> p h d", d=64)
                P.tt(V(sq[:, :], "sq"), V(pt[:, 0:1280], "pt"), V(pt[:, 0:1280], "pt"), ALU.mult)
                P.reduce(V(ss[:, :], "ss"), V(sq[:, :].rearrange("p (h d) -> p h d", d=64), "sq"), ALU.add)
                rsqrt_(P, V(ss[:, :], "ss"), V(ss[:, :], "ss"), 1.0 / 64, C.eps_t)
                P.tt(V(xn[:, :, :], "xn"), V(x3, "pt"), V(ss[:, :].unsqueeze(2).to_broadcast([128, 20, 64]), "ss"), ALU.mult)
                P.tt(V(xn[:, :, :], "xn"), V(xn[:, :, :], "xn"), V(w20[:, :, :], "w20"), ALU.mult)
                cosb = V(cst[:, 0:32].unsqueeze(1).to_broadcast([128, 20, 32]), "cst")
                sinb = V(cst[:, 32:64].unsqueeze(1).to_broadcast([128, 20, 32]), "cst")
                P.tt(V(ro[:, :, 0:32], "ro"), V(xn[:, :, 0:32], "xn"), cosb, ALU.mult)
                P.tt(V(t1[:, :, :], "t1"), V(xn[:, :, 32:64], "xn"), sinb, ALU.mult)
                P.tt(V(ro[:, :, 0:32], "ro"), V(ro[:, :, 0:32], "ro"), V(t1[:, :, :], "t1"), ALU.subtract)
                P.tt(V(ro[:, :, 32:64], "ro"), V(xn[:, :, 32:64], "xn"), cosb, ALU.mult)
                P.tt(V(t1[:, :, :], "t1"), V(xn[:, :, 0:32], "xn"), sinb, ALU.mult)
                P.tt(V(ro[:, :, 32:64], "ro"), V(ro[:, :, 32:64], "ro"), V(t1[:, :, :], "t1"), ALU.add)
                P.copy(V(rob[:, :, :], "rob"), V(ro[:, :, :], "ro"), e="act")
                P.copy(V(ptb[:, :], "ptb"), V(pt[:, 0:1280], "pt"), e="act")
                P.copy(V(svp[:, tt, :, 0:64], "svp"), V(pt[:, 896:1024].rearrange("p (g d) -> p g d", d=64), "pt"), e="pool")
                P.copy(V(wvp[:, tt, :, 0:64], "wvp"), V(pt[:, 1152:1280].rearrange("p (g d) -> p g d", d=64), "pt"), e="pool")
                P.act(V(gates[:, tt, :], "gates"), V(pt[:, 1280:1304], "pt"), AF.Sigmoid)
                for b in range(2):
                    pb, pk = C.psum()
                    for c4 in range(4):
                        hh = b * 4 + c4
                        P.mm(V(pb[0:64, c4 * 128:(c4 + 1) * 128], pk), V(rob[:, hh, :], "rob"), V(identb[:, :], "identb"), inc=(c4 == 3))
                    P.copy(V(qTt[:, b * 4:(b + 1) * 4, :], "qTt"), V(pb[0:64, :].rearrange("p (c t) -> p c t", c=4), pk), e="act")
                P.dma(V(C.qT_d[:, :, tt * 128:(tt + 1) * 128].rearrange("h d t -> d h t"), "qT_d%d" % tt), V(qTt[:, :, :], "qTt"), q="act")
                pb, pk = C.psum()
                for c4, hh in enumerate((12, 13, 16, 17)):
                    P.mm(V(pb[0:64, c4 * 128:(c4 + 1) * 128], pk), V(rob[:, hh, :], "rob"), V(identb[:, :], "identb"), inc=(c4 == 3))
                P.copy(V(kTt[:, :, :], "kTt"), V(pb[0:64, :].rearrange("p (c t) -> p c t", c=4), pk), e="dve")
                P.dma(V(C.kT_d[:, :, tt * 128:(tt + 1) * 128].rearrange("h d t -> d h t"), "kT_d%d" % tt), V(kTt[:, :, :], "kTt"), q="act")
                pb, pk = C.psum()
                for c4 in range(4):
                    P.mm(V(pb[0:64, c4 * 128:(c4 + 1) * 128], pk), V(ptb[:, 512 + c4 * 64:512 + (c4 + 1) * 64], "ptb"), V(identb[:, :], "identb"), inc=(c4 == 3))
                P.copy(V(ccT[:, :, tt * 128:(tt + 1) * 128], "ccT"), V(pb[0:64, :].rearrange("p (c t) -> p c t", c=4), pk), e="dve")
            w1 = sb("n_w1", [64, 32, 64], BF16)
            w1f = sb("n_w1f", [128, 16, 64])
            posf = sb("n_posf", [128, 16])
            w2 = sb("n_w2", [64, 64], BF16)
            bia = sb("n_bia", [64, 1])
            h1 = sb("n_h1", [64, NCT * 128], BF16)
            P.memset(V(h1[:, :], "h1"), 0.0)
            kw0 = sb("n_kw0", [128, 64])
            P.dma(V(kw0[:, :], "kw0"), C.k_norm[l, 0:1, :].partition_broadcast(128))
            csc = sb("n_csc", [128, NCT, 64])
            P.memset(V(csc[:, :, :], "csc"), 0.0)
            for ct in range(NCT):
                n = min(128, NCMP - ct * 128)
                r0 = 31 + 16 * ct * 128
                P.dma(V(csc[0:n, ct, :], "csc"), V(C.cs_d[r0:r0 + 16 * (n - 1) + 1:16, :], "cs_all"))
            kc = sb("n_kc", [128, 64])
            kc2 = sb("n_kc2", [128, 64])
            kss = sb("n_kss", [128, 1])
            kt1 = sb("n_kt1", [128, 32])
            kcb = sb("n_kcb", [128, 64], BF16)
            for j in range(2):
                for g in range(2):
                    src = ccT[:, j * 2 + g, :]
                    P.dma(V(w1[:, :, :], "w1"), C.w_cmp1[l, j].rearrange("(l d) o -> d l o", d=64), q="pool")
                    P.dma(V(w1f[:, :, :], "w1f"), C.w_cmp1[l, j].rearrange("(c p) o -> p c o", p=128))
                    P.dma(V(posf[:, :], "posf"), C.cmp_pos[l, j].rearrange("(c a) d -> (a d) c", a=2))
                    P.dma(V(w2[:, :], "w2"), C.w_cmp2[l, j], q="pool")
                    pbb, pbk = C.psum()
                    for c in range(16):
                        P.mm(V(pbb[0:64, 0:1], pbk), V(w1f[:, c, :], "w1f"), V(posf[:, c:c + 1], "posf"), start=(c == 0), stop=(c == 15))
                    P.copy(V(bia[:, :], "bia"), V(pbb[0:64, 0:1], pbk))
                    pb, pk = C.psum()
                    for ll in range(32):
                        P.mm(V(pb[0:64, 0:NCMP], pk), V(w1[:, ll, :], "w1"), V(src[:, ll:ll + 16 * (NCMP - 1) + 1:16], "ccT"),
                             start=(ll == 0), stop=(ll == 31))
                    P.act(V(h1[:, 0:NCMP], "h1"), V(pb[0:64, 0:NCMP], pk), AF.Silu, bias=V(bia[:, :], "bia"))
                    for ct in range(NCT):
                        po, pok = C.psum()
                        P.mm(V(po[:, 0:64], pok), V(h1[:, ct * 128:(ct + 1) * 128], "h1"), V(w2[:, :], "w2"))
                        if j == 1:
                            P.copy(V(rhsc[:, ct, g, 0:64], "rhsc"), V(po[:, 0:64], pok))
                        else:
                            P.copy(V(kc[:, :], "kc"), V(po[:, 0:64], pok))
                            P.tt(V(kc2[:, :], "kc2"), V(kc[:, :], "kc"), V(kc[:, :], "kc"), ALU.mult)
                            P.reduce(V(kss[:, :], "kss"), V(kc2[:, :], "kc2"), ALU.add)
                            rsqrt_(P, V(kss[:, :], "kss"), V(kss[:, :], "kss"), 1.0 / 64, C.eps_t)
                            P.stt(V(kc[:, :], "kc"), V(kc[:, :], "kc"), V(kss[:, 0:1], "kss"), V(kw0[:, :], "kw0"), ALU.mult, ALU.mult)
                            cc_, sc_ = V(csc[:, ct, 0:32], "csc"), V(csc[:, ct, 32:64], "csc")
                            P.tt(V(kc2[:, 0:32], "kc2"), V(kc[:, 0:32], "kc"), cc_, ALU.mult)
                            P.tt(V(kt1[:, :], "kt1"), V(kc[:, 32:64], "kc"), sc_, ALU.mult)
                            P.tt(V(kc2[:, 0:32], "kc2"), V(kc2[:, 0:32], "kc2"), V(kt1[:, :], "kt1"), ALU.subtract)
                            P.tt(V(kc2[:, 32:64], "kc2"), V(kc[:, 32:64], "kc"), cc_, ALU.mult)
                            P.tt(V(kt1[:, :], "kt1"), V(kc[:, 0:32], "kc"), sc_, ALU.mult)
                            P.tt(V(kc2[:, 32:64], "kc2"), V(kc2[:, 32:64], "kc2"), V(kt1[:, :], "kt1"), ALU.add)
                            P.copy(V(kcb[:, :], "kcb"), V(kc2[:, :], "kc2"))
                            pq, pqk = C.psum()
                            P.mm(V(pq[0:64, 0:128], pqk), V(kcb[:, :], "kcb"), V(identb[:, :], "identb"))
                            P.copy(V(kcT[:, g, ct * 128:(ct + 1) * 128], "kcT"), V(pq[0:64, 0:128], pqk))
            P.barrier()
        P.es = es0
        stage3b(P, C, l, svp, wvp, gates, rhsc, kcT, identb)
        P.barrier()
        P.es = old


def stage3b(P, C, l, svp, wvp, gates, rhsc, kcT, identb):
    S = C.S
    NT = S // 128
    NCMP = (S - 32) // 16 + 1
    NCT = (NCMP + 127) // 128
    with contextlib.ExitStack() as es:
        P.es = es
        sb = P.sb
        kTa = sb("b_kTa", [64, 4, S], BF16)
        for i in range(4):
            P.dma(V(kTa[:, i, :], "kTa"), V(C.kT_d[i], "kT_d_all"))
        Ex = sb("b_Ex", [128, NT, 128], BF16)
        Exf = sb("b_Exf", [128, 128])
        for kt in range(NT):
            P.dma(V(Exf[:, :], "Exf"), C.k_Ex[:, kt, :])
            P.copy(V(Ex[:, kt, :], "Ex"), V(Exf[:, :], "Exf"))
        cf = sb("b_cf", [128, 2304 + 128])
        Mc = sb("b_Mc", [128, 2304 + 128], BF16)
        P.dma(V(cf[:, :], "cf"), C.k_Mc)
        P.copy(V(Mc[:, :], "Mc"), V(cf[:, :], "cf"))
        Km = sb("b_Km", [128, 256])
        Fm = sb("b_Fm", [128, 256])
        P.dma(V(Km[:, :], "Km"), C.k_Km)
        P.dma(V(Fm[:, :], "Fm"), C.k_Fm)
        Dm = sb("b_D", [128, 128], BF16)
        Dn = sb("b_Dn", [128, 128], BF16)
        P.dma(V(cf[:, 0:128], "cf"), C.k_U)
        P.copy(V(Dm[:, :], "D"), V(cf[:, 0:128], "cf"))
        P.ts(V(cf[:, 0:128], "cf"), V(cf[:, 0:128], "cf"), -1.0, ALU.mult, 1.0, ALU.add)
        P.copy(V(Dn[:, :], "Dn"), V(cf[:, 0:128], "cf"))
        qTq = sb("b_qTq", [64, 8, 128], BF16)
        ecp = [sb("b_ec%d" % i, [128, 128], BF16) for i in range(NCT)]
        impacc = sb("b_imp", [128, 128])
        impadj = sb("b_impadj", [128, 128])
        imr = sb("b_imr", [128, 128])
        m8a = sb("b_m8a", [128, 8])
        m8b = sb("b_m8b", [128, 8])
        sel = sb("b_sel", [128, 128], BF16)
        selT = sb("b_selT", [128, 128], BF16)
        rz = sb("b_rz", [128, 1])
        gz = sb("b_gz", [128, 1])
        ob = sb("b_ob", [128, 8, 64])
        mskp = Pool(P, "b_msk", 2, [128, 128], BF16)
        ep = Pool(P, "b_e", 3, [128, 128], BF16)
        emp = Pool(P, "b_em", 3, [128, 128], BF16)

        def finish_head(pacc, pk_, hh, gi, first):
            c0 = 0
            P.ts(V(rz[:, :], "rz"), V(pacc[:, c0 + 64:c0 + 65], pk_), 1e-30, ALU.max)
            P.op("dve", lambda v: v.reciprocal(rz[:, :], rz[:, :]), reads=[V(rz[:, :], "rz")], writes=[V(rz[:, :], "rz")])
            P.tt(V(gz[:, :], "gz"), V(rz[:, :], "rz"), V(gates[:, C._qt, hh * 3 + gi:hh * 3 + gi + 1], "gates"), ALU.mult)
            if first:
                P.ts(V(ob[:, hh, :], "ob"), V(pacc[:, c0:c0 + 64], pk_), V(gz[:, 0:1], "gz"), ALU.mult)
            else:
                P.stt(V(ob[:, hh, :], "ob"), V(pacc[:, c0:c0 + 64], pk_), V(gz[:, 0:1], "gz"), V(ob[:, hh, :], "ob"), ALU.mult, ALU.add)

        C.psn = 4
        for qt in range(NT):
            C._qt = qt
            t0 = qt * 128
            P.dma(V(qTq[:, :, :], "qTq"), V(C.qT_d[:, :, t0:t0 + 128].rearrange("h d t -> d h t"), "qT_d_all"))
            for g in range(2):
                cts = [ct for ct in range(NCT) if t0 - 2048 * ct >= 0]
                for h in range(4):
                    hh = g * 4 + h
                    for ct in cts:
                        o_ = t0 - 2048 * ct
                        ps, psk = C.psum()
                        P.mm(V(ps[:, 0:128], psk), V(kcT[:, g, ct * 128:(ct + 1) * 128], "kcT"), V(qTq[:, hh, :], "qTq"))
                        P.act(V(ecp[ct][:, :], "ec%d" % ct), V(ps[:, 0:128], psk), AF.Exp)
                        if o_ < 2304:
                            P.tt(V(ecp[ct][:, :], "ec%d" % ct), V(ecp[ct][:, :], "ec%d" % ct), V(Mc[:, o_:o_ + 128], "Mc"), ALU.mult)
                    pc, pck = C.psum()
                    for i, ct in enumerate(cts):
                        P.mm(V(pc[:, 0:193], pck), V(ecp[ct][:, :], "ec%d" % ct), V(rhsc[:, ct, g, :], "rhsc"),
                             start=(i == 0), stop=(i == len(cts) - 1))
                    pcv = V(pc[:, :], pck)
                    P.ts(V(rz[:, :], "rz"), V(pc[:, 64:65], pck), 1e-30, ALU.max)
                    P.op("dve", lambda v: v.reciprocal(rz[:, :], rz[:, :]), reads=[V(rz[:, :], "rz")], writes=[V(rz[:, :], "rz")])
                    if h == 0:
                        P.ts(V(impacc[:, :], "imp"), V(pc[:, 65:193], pck), V(rz[:, 0:1], "rz"), ALU.mult)
                    else:
                        P.stt(V(impacc[:, :], "imp"), V(pc[:, 65:193], pck), V(rz[:, 0:1], "rz"), V(impacc[:, :], "imp"), ALU.mult, ALU.add)
                    P.tt(V(gz[:, :], "gz"), V(rz[:, :], "rz"), V(gates[:, qt, hh * 3:hh * 3 + 1], "gates"), ALU.mult)
                    P.ts(V(ob[:, hh, :], "ob"), V(pc[:, 0:64], pck), V(gz[:, 0:1], "gz"), ALU.mult)
                off = 128 - 2 * qt
                P.tt(V(impadj[:, :], "impadj"), V(impacc[:, :], "imp"), V(Km[:, off:off + 128], "Km"), ALU.mult)
                P.tt(V(impadj[:, :], "impadj"), V(impadj[:, :], "impadj"), V(Fm[:, off:off + 128], "Fm"), ALU.add)
                P.memset(V(impadj[:, 0:1], "impadj"), FORCE + 128.0)
                P.op("dve", lambda v: v.max(m8a[:, :], impadj[:, :]), reads=[V(impadj[:, :], "impadj")], writes=[V(m8a[:, :], "m8a")])
                P.op("dve", lambda v: v.match_replace(imr[:, :], m8a[:, :], impadj[:, :], -3.0e6),
                     reads=[V(impadj[:, :], "impadj"), V(m8a[:, :], "m8a")], writes=[V(imr[:, :], "imr")])
                P.op("dve", lambda v: v.max(m8b[:, :], imr[:, :]), reads=[V(imr[:, :], "imr")], writes=[V(m8b[:, :], "m8b")])
                P.ts(V(sel[:, :], "sel"), V(impadj[:, :], "impadj"), V(m8b[:, 7:8], "m8b"), ALU.is_ge)
                pt_, ptk = C.psum()
                P.mm(V(pt_[:, 0:128], ptk), V(sel[:, :], "sel"), V(identb[:, :], "identb"))
                P.copy(V(selT[:, :], "selT"), V(pt_[:, 0:128], ptk))
                for br in range(2):
                    kbase = 0 if br == 0 else 2
                    vp = svp if br == 0 else wvp
                    vk = "svp" if br == 0 else "wvp"
                    kts = list(range(0, qt + 1)) if br == 0 else [kt for kt in range(qt - 4, qt + 1) if kt >= 0]
                    for i, kt in enumerate(kts):
                        mk = None
                        if br == 0:
                            pm, pmk = C.psum()
                            P.mm(V(pm[:, 0:128], pmk), V(Ex[:, kt, :], "Ex"), V(selT[:, :], "selT"))
                            msk, mskk = mskp.next()
                            P.copy(V(msk[:, :], mskk), V(pm[:, 0:128], pmk), e="act")
                            if kt == qt:
                                P.tt(V(msk[:, :], mskk), V(msk[:, :], mskk), V(Dm[:, :], "D"), ALU.mult, e="pool")
                            mk = V(msk[:, :], mskk)
                        else:
                            if kt == qt:
                                mk = V(Dm[:, :], "D")
                            elif kt == qt - 4:
                                mk = V(Dn[:, :], "Dn")
                        for h in range(4):
                            hh = g * 4 + h
                            ps, psk = C.psum()
                            P.mm(V(ps[:, 0:128], psk), V(kTa[:, kbase + g, kt * 128:(kt + 1) * 128], "kTa"), V(qTq[:, hh, :], "qTq"))
                            e_, ek = ep.next()
                            P.act(V(e_[:, :], ek), V(ps[:, 0:128], psk), AF.Exp)
                            if mk is not None:
                                em, emk = emp.next()
                                P.tt(V(em[:, :], emk), V(e_[:, :], ek), mk, ALU.mult)
                            else:
                                em, emk = e_, ek
                            P.mm(V(C.psb[4 + h][:, 0:65], "pacc%d" % h), V(em[:, :], emk), V(vp[:, kt, g, :], vk),
                                 start=(i == 0), stop=(i == len(kts) - 1), inc=True)
                    for h in range(4):
                        finish_head(C.psb[4 + h], "pacc%d" % h, g * 4 + h, 1 + br, False)
            P.dma(V(C.ob_d[t0:t0 + 128, :], "ob_d%d" % qt), V(ob[:, :, :].rearrange("p h d -> p (h d)"), "ob"), q="act")
        C.psn = 8


def stage4(P, C, l):
    S = C.S
    with contextlib.ExitStack() as es:
        P.es, old = es, P.es
        sb = P.sb
        nsc, nsh, grow = load_layer_vecs(P, C, l, "m")
        identb = sb("m_identb", [128, 128], BF16)
        P.copy(V(identb[:, :], "identb"), C.ident)
        Woa = sb("m_Woa", [128, 4, D], BF16)
        Wob = sb("m_Wob", [128, 4, D], BF16)
        Wout = sb("m_Wout", [128, 8, D], BF16)
        for h2 in range(2):
            cs_ = slice(h2 * 512, (h2 + 1) * 512)
            P.dma(V(Woa[:, :, cs_], "Woa"), C.w_oa[l, :, cs_].rearrange("(c p) n -> p c n", p=128), q="pool")
            P.dma(V(Wob[:, :, cs_], "Wob"), C.w_ob[l, :, cs_].rearrange("(c p) n -> p c n", p=128), q="pool")
            P.dma(V(Wout[:, :, cs_], "Wout"), C.w_out[l, :, cs_].rearrange("(c p) n -> p c n", p=128), q="pool")
        oab = sb("m_oab", [128, 2, 512])
        oabb = sb("m_oabb", [128, 2, 512], BF16)
        oT = sb("m_oT", [128, 8, 128], BF16)
        mg = sb("m_mg", [128, 2048])
        xt = sb("m_xt", [128, D])
        t1 = sb("m_t1", [128, D])
        t2 = sb("m_t2", [128, D])
        ymb = sb("m_ymb", [128, D], BF16)
        yT = sb("m_yT", [128, 8, 128], BF16)
        xo = sb("m_xo", [128, D])
        for tt in range(S // 128):
            r = slice(tt * 128, (tt + 1) * 128)
            P.dma(V(oab[:, 0, :], "oab"), V(C.oa_d[r, :], "oa_all"))
            P.dma(V(oab[:, 1, :], "oab"), V(C.ob_d[r, :], "ob_all"))
            P.dma(V(mg[:, :], "mg"), V(C.proj_d[r, O_MG:O_MG + 2048], "proj_all"))
            P.dma(V(xt[:, :], "xt"), V(C.xcur[r, :], "x_all"))
            P.copy(V(oabb[:, :, :], "oabb"), V(oab[:, :, :], "oab"), e="pool")
            P.act(V(mg[:, :], "mg"), V(mg[:, :], "mg"), AF.Sigmoid)
            for ab in range(2):
                pb, pk = C.psum()
                for c4 in range(4):
                    P.mm(V(pb[:, c4 * 128:(c4 + 1) * 128], pk), V(oabb[:, ab, c4 * 128:(c4 + 1) * 128], "oabb"), V(identb[:, :], "identb"), inc=(c4 == 3))
                P.copy(V(oT[:, ab * 4:(ab + 1) * 4, :], "oT"), V(pb[:, :].rearrange("p (c t) -> p c t", c=4), pk), e="act")
            for ab in range(2):
                Wm, wk = (Woa, "Woa") if ab == 0 else (Wob, "Wob")
                tdst = t1 if ab == 0 else t2
                for h2 in range(2):
                    pb, pk = C.psum()
                    for k in range(4):
                        P.mm(V(pb[:, :], pk), V(oT[:, ab * 4 + k, :], "oT"), V(Wm[:, k, h2 * 512:(h2 + 1) * 512], wk), start=(k == 0), stop=(k == 3))
                    P.tt(V(tdst[:, h2 * 512:(h2 + 1) * 512], "t%d" % ab), V(pb[:, :], pk),
                         V(mg[:, ab * 1024 + h2 * 512:ab * 1024 + (h2 + 1) * 512], "mg"), ALU.mult)
            P.tt(V(ymb[:, :], "ymb"), V(t1[:, :], "t0"), V(t2[:, :], "t1"), ALU.add, e="pool")
            for half in range(2):
                pb, pk = C.psum()
                for c4 in range(4):
                    c = half * 4 + c4
                    P.mm(V(pb[:, c4 * 128:(c4 + 1) * 128], pk), V(ymb[:, c * 128:(c + 1) * 128], "ymb"), V(identb[:, :], "identb"), inc=(c4 == 3))
                P.copy(V(yT[:, half * 4:(half + 1) * 4, :], "yT"), V(pb[:, :].rearrange("p (c t) -> p c t", c=4), pk), e="act")
            for h2 in range(2):
                pb, pk = C.psum()
                for k in range(8):
                    P.mm(V(pb[:, :], pk), V(yT[:, k, :], "yT"), V(Wout[:, k, h2 * 512:(h2 + 1) * 512], "Wout"), start=(k == 0), stop=(k == 7))
                P.tt(V(xo[:, h2 * 512:(h2 + 1) * 512], "xo"), V(pb[:, :], pk), V(_ap(grow)[:, h2 * 512:(h2 + 1) * 512], grow.key), ALU.mult)
            P.tt(V(xo[:, :], "xo"), V(xo[:, :], "xo"), V(xt[:, :], "xt"), ALU.add, e="pool")
            P.dma(V(C.xmid_d[r, :], "xmid%d" % tt), V(xo[:, :], "xo"), q="act")
        P.barrier()
        P.es = old


def stage5(P, C, l, xout):
    S = C.S
    moe = (l % 2 == 1)
    li = l // 2
    TS = min(8, S // 128)
    T = TS * 128
    NH = (T + 511) // 512
    HW = min(512, T)
    with contextlib.ExitStack() as es:
        P.es, old = es, P.es
        sb = P.sb
        nsc, nsh, grow = load_layer_vecs(P, C, l, "f")
        C.p_ss = Pool(P, "f_ss", 2, [128, 1])
        C.p_sq = Pool(P, "f_sq", 2, [128, D])
        xp = Pool(P, "f_x", 2, [128, D])
        hT = sb("f_hT", [128, 8, T], BF16)
        acc = sb("f_acc", [128, TS, D])
        gT = sb("f_gT", [128, 4, T], BF16)
        sa = sb("f_sa", [128, 512])
        w1p = Pool(P, "f_w1", 2, [128, 8, 512], BF16)
        w3p = Pool(P, "f_w3", 2, [128, 8, 512], BF16)
        w2p = Pool(P, "f_w2", 2, [128, 4, D], BF16)
        xo = sb("f_xo", [128, D])
        if moe:
            Wr = sb("f_Wr", [128, 8, NEXP])
            P.dma(V(Wr[:, :, :], "Wr"), C.w_router[li].rearrange("(c p) e -> p c e", p=128))
            h32 = sb("f_h32", [128, 8, 128])
            lg = sb("f_lg", [128, NEXP])
            m8 = sb("f_m8", [128, 8])
            nt1 = sb("f_nt1", [128, 1])
            msk = sb("f_msk", [128, NEXP])
            ex = sb("f_ex", [128, NEXP])
            den = sb("f_den", [128, 1])
            comb = sb("f_comb", [128, TS, NEXP])
        nexp = NEXP if moe else 1
        for st in range(S // T):
            for sub in range(TS):
                tt = st * TS + sub
                xt, kx = xp.next()
                P.dma(V(xt[:, :], kx), V(C.xmid_d[tt * 128:(tt + 1) * 128, :], "xmid_all"))
                norm_mod_T(P, C, xt, kx, nsc, nsh, hT, "hT", tok0=sub * 128, hT32=((h32, "h32") if moe else None))
                if moe:
                    pb, pk = C.psum()
                    for k in range(8):
                        P.mm(V(pb[:, 0:NEXP], pk), V(h32[:, k, :], "h32"), V(Wr[:, k, :], "Wr"), start=(k == 0), stop=(k == 7))
                    P.copy(V(lg[:, :], "lg"), V(pb[:, 0:NEXP], pk))
                    P.op("dve", lambda v: v.max(m8[:, :], lg[:, :]), reads=[V(lg[:, :], "lg")], writes=[V(m8[:, :], "m8")])
                    P.ts(V(nt1[:, :], "nt1"), V(m8[:, 0:1], "m8"), -1.0, ALU.mult)
                    P.ts(V(msk[:, :], "msk"), V(lg[:, :], "lg"), V(m8[:, 1:2], "m8"), ALU.is_ge)
                    P.act(V(ex[:, :], "ex"), V(lg[:, :], "lg"), AF.Exp, bias=V(nt1[:, :], "nt1"))
                    P.tt(V(ex[:, :], "ex"), V(ex[:, :], "ex"), V(msk[:, :], "msk"), ALU.mult)
                    P.reduce(V(den[:, :], "den"), V(ex[:, :], "ex"), ALU.add)
                    P.op("dve", lambda v: v.reciprocal(den[:, :], den[:, :]), reads=[V(den[:, :], "den")], writes=[V(den[:, :], "den")])
                    P.ts(V(comb[:, sub, :], "comb"), V(ex[:, :], "ex"), V(den[:, 0:1], "den"), ALU.mult)
            for e in range(nexp):
                for j in range(FFN // 512):
                    fs = slice(j * 512, (j + 1) * 512)
                    w1, k1 = w1p.next()
                    w3, k3 = w3p.next()
                    w2, k2 = w2p.next()
                    if moe:
                        s1, s3, s2 = C.w1_moe[li, e, :, fs], C.w3_moe[li, e, :, fs], C.w2_moe[li, e, fs, :]
                    else:
                        s1, s3, s2 = C.w1_dense[li, :, fs], C.w3_dense[li, :, fs], C.w2_dense[li, fs, :]
                    P.dma(V(w1[:, :, :], k1), s1.rearrange("(c p) n -> p c n", p=128), q="pool")
                    P.dma(V(w3[:, :, :], k3), s3.rearrange("(c p) n -> p c n", p=128), q="pool")
                    for h2 in range(2):
                        P.dma(V(w2[:, :, h2 * 512:(h2 + 1) * 512], k2), s2[:, h2 * 512:(h2 + 1) * 512].rearrange("(c p) n -> p c n", p=128), q="pool")
                    for i in range(4):
                        for nh in range(NH):
                            ts_ = slice(nh * 512, nh * 512 + HW)
                            pa, pak = C.psum()
                            for k in range(8):
                                P.mm(V(pa[:, 0:HW], pak), V(w1[:, k, i * 128:(i + 1) * 128], k1), V(hT[:, k, ts_], "hT"), start=(k == 0), stop=(k == 7))
                            pb, pbk = C.psum()
                            for k in range(8):
                                P.mm(V(pb[:, 0:HW], pbk), V(w3[:, k, i * 128:(i + 1) * 128], k3), V(hT[:, k, ts_], "hT"), start=(k == 0), stop=(k == 7))
                            P.act(V(sa[:, 0:HW], "sa"), V(pa[:, 0:HW], pak), AF.Silu)
                            P.tt(V(gT[:, i, ts_], "gT"), V(pb[:, 0:HW], pbk), V(sa[:, 0:HW], "sa"), ALU.mult)
                    first = (e == 0 and j == 0)
                    for sub in range(TS):
                        for h2 in range(2):
                            po, pok = C.psum()
                            for i in range(4):
                                P.mm(V(po[:, :], pok), V(gT[:, i, sub * 128:(sub + 1) * 128], "gT"), V(w2[:, i, h2 * 512:(h2 + 1) * 512], k2), start=(i == 0), stop=(i == 3))
                            a_ = V(acc[:, sub, h2 * 512:(h2 + 1) * 512], "acc")
                            if moe:
                                cb = V(comb[:, sub, e:e + 1], "comb")
                                if first:
                                    P.ts(a_, V(po[:, :], pok), cb, ALU.mult)
                                else:
                                    P.stt(a_, V(po[:, :], pok), cb, a_, ALU.mult, ALU.add)
                            else:
                                if first:
                                    P.copy(a_, V(po[:, :], pok))
                                else:
                                    P.tt(a_, V(po[:, :], pok), a_, ALU.add)
            for sub in range(TS):
                tt = st * TS + sub
                xt, kx = xp.next()
                P.dma(V(xt[:, :], kx), V(C.xmid_d[tt * 128:(tt + 1) * 128, :], "xmid_all"))
                P.tt(V(xo[:, :], "xo"), V(acc[:, sub, :], "acc"), grow, ALU.mult)
                P.tt(V(xo[:, :], "xo"), V(xo[:, :], "xo"), V(xt[:, :], kx), ALU.add, e="pool")
                P.dma(V(xout[tt * 128:(tt + 1) * 128, :], "xout%d" % tt), V(xo[:, :], "xo"), q="act")
        P.barrier()
        P.es = old


_PROG_CACHE = {}


def kernel(**inputs):
    x = np.asarray(inputs["x"])
    B, S, _ = x.shape
    L = int(np.asarray(inputs["w_ada"]).shape[0])
    key = (S, L)
    if key not in _PROG_CACHE:
        _PROG_CACHE[key] = build_program(S, L)
    nc = _PROG_CACHE[key]
    consts = host_consts(S)
    shared = {}
    for k, v in inputs.items():
        if k in ("x", "c", "positions"):
            continue
        shared[k] = np.ascontiguousarray(np.asarray(v))
    shared.update(consts)
    in_maps = []
    for b in range(B):
        m = dict(shared)
        m["x"] = np.ascontiguousarray(x[b])
        m["c"] = np.ascontiguousarray(np.asarray(inputs["c"])[b])
        m["positions"] = np.ascontiguousarray(np.asarray(inputs["positions"])[b].astype(np.int32))
        in_maps.append(m)
    res = run_bass_kernel_spmd(nc, in_maps, core_ids=list(range(B)))
    return np.stack([np.asarray(r["out"]) for r in res.results], axis=0).astype(np.float32)
```

```python
import contextlib
import numpy as np
import concourse.bass as bass
import concourse.mybir as mybir

F32 = mybir.dt.float32
BF16 = mybir.dt.bfloat16
I32 = mybir.dt.int32
ALU = mybir.AluOpType
AF = mybir.ActivationFunctionType
AX = mybir.AxisListType

NRING = 24


class V:
    __slots__ = ("ap", "key")

    def __init__(self, ap, key):
        self.ap = ap
        self.key = key


def _ap(x):
    return x.ap if isinstance(x, V) else x


class Prog:
    def __init__(self, nc, es):
        self.nc = nc
        self.es = es
        self.eng = {"pe": nc.tensor, "act": nc.scalar, "dve": nc.vector,
                    "pool": nc.gpsimd, "sp": nc.sync}
        self.sem = {e: es.enter_context(nc.semaphore("s_" + e)) for e in self.eng}
        self.cnt = {e: 0 for e in self.eng}
        self.pend = {e: False for e in self.eng}
        self.waited = {e: {} for e in self.eng}
        self.ring = [es.enter_context(nc.semaphore("r%d" % i)) for i in range(2 * NRING)]
        self.dma_n = [0, 0]
        self.state = {}
        self.uid = 0
        self.ninst = 0

    def sb(self, name, shape, dt=F32):
        self.uid += 1
        name = "%s_u%d" % (name, self.uid)
        t = self.es.enter_context(self.nc.sbuf_tensor(name, list(shape), dt))
        return t

    def ps(self, name, shape, dt=F32):
        t = self.es.enter_context(self.nc.psum_tensor(name, list(shape), dt))
        return t

    def dram(self, name, shape, dt=F32, kind="Internal"):
        return self.nc.dram_tensor(name, list(shape), dt, kind=kind)

    def key(self, base="k"):
        self.uid += 1
        return "%s%d" % (base, self.uid)

    def _st(self, k):
        s = self.state.get(k)
        if s is None:
            s = {"w": {}, "wd": [], "r": {}, "rd": []}
            self.state[k] = s
        return s

    def _wait(self, e, tok):
        eng = self.eng[e]
        if tok[0] == "c":
            _, e2, idx = tok
            if self.waited[e].get(e2, 0) >= idx:
                return
            if e2 == e and (e == "pe" or idx > self.cnt[e2]):
                return
            if idx > self.cnt[e2]:
                raise RuntimeError("wait on un-emitted inc: %s waits %s idx %d cnt %d" % (e, e2, idx, self.cnt[e2]))
            eng.wait_ge(self.sem[e2], idx)
            self.waited[e][e2] = idx
        else:
            _, slot, val = tok
            kk = ("d", slot)
            if self.waited[e].get(kk, 0) >= val:
                return
            eng.wait_ge(self.ring[slot], val)
            self.waited[e][kk] = val

    def _deps(self, e, reads, writes):
        toks = []
        for v in reads:
            if isinstance(v, V) and v.key is not None:
                s = self._st(v.key)
                toks += [("c", e2, i) for e2, i in s["w"].items()]
                toks += s["wd"]
        for v in writes:
            if isinstance(v, V) and v.key is not None:
                s = self._st(v.key)
                toks += [("c", e2, i) for e2, i in s["w"].items()]
                toks += s["wd"]
                toks += [("c", e2, i) for e2, i in s["r"].items()]
                toks += s["rd"]
        for t in toks:
            self._wait(e, t)

    def _record(self, tok, reads, writes):
        for v in reads:
            if isinstance(v, V) and v.key is not None:
                s = self._st(v.key)
                if tok[0] == "c":
                    s["r"][tok[1]] = tok[2]
                else:
                    s["rd"].append(tok)
        for v in writes:
            if isinstance(v, V) and v.key is not None:
                s = self._st(v.key)
                if tok[0] == "c":
                    s["w"] = {tok[1]: tok[2]}
                    s["wd"] = []
                else:
                    s["w"] = {}
                    s["wd"] = [tok]
                s["r"] = {}
                s["rd"] = []

    def op(self, e, fn, reads=(), writes=(), inc=True):
        self._deps(e, reads, writes)
        ins = fn(self.eng[e])
        self.ninst += 1
        if inc:
            self.cnt[e] += 1
            ins.then_inc(self.sem[e], 1)
            tok = ("c", e, self.cnt[e])
            self.pend[e] = False
        else:
            tok = ("c", e, self.cnt[e] + 1)
            self.pend[e] = True
        self._record(tok, reads, writes)
        return ins

    def dma(self, out, in_, q="sp", **kw):
        self._deps(q, [in_], [out])
        r = 1 if q == "pool" else 0
        slot = r * NRING + self.dma_n[r] % NRING
        val = 16 * (self.dma_n[r] // NRING + 1)
        if val > 16:
            self._wait(q, ("d", slot, val - 16))
        self.dma_n[r] += 1
        ins = self.eng[q].dma_start(out=_ap(out), in_=_ap(in_), **kw)
        ins.then_inc(self.ring[slot], 16)
        self.ninst += 1
        self._record(("d", slot, val), [in_], [out])
        return ins

    def barrier(self):
        for e in self.eng:
            if self.pend[e]:
                raise RuntimeError("pending un-inc'ed instruction on " + e)
        for e2 in self.eng:
            if e2 != "pool" and self.cnt[e2] > 0:
                self._wait("pool", ("c", e2, self.cnt[e2]))
        for r in range(2):
            lo = max(0, self.dma_n[r] - NRING)
            for i in range(lo, self.dma_n[r]):
                self._wait("pool", ("d", r * NRING + i % NRING, 16 * (i // NRING + 1)))
        self.op("pool", lambda g: g.memset(self._bar_t[:, :], 0.0), writes=[V(self._bar_t[:, :], "bar_t")])
        tok = ("c", "pool", self.cnt["pool"])
        for e in self.eng:
            if e != "pool":
                self._wait(e, tok)
                for e2 in self.eng:
                    self.waited[e][e2] = max(self.waited[e].get(e2, 0), self.cnt[e2] if e2 != "pool" else self.cnt["pool"])
        for e2 in self.eng:
            if e2 != "pool":
                self.waited["pool"][e2] = self.cnt[e2]
        self.state = {"bar_t": {"w": {"pool": self.cnt["pool"]}, "wd": [], "r": {}, "rd": []}}

    def init(self):
        self._bar_t = self.sb("bar_t", [128, 8], F32)

    def mm(self, out, lhsT, rhs, start=True, stop=True, inc=None):
        if inc is None:
            inc = stop
        return self.op("pe", lambda t: t.matmul(_ap(out), _ap(lhsT), _ap(rhs), start=start, stop=stop),
                       reads=[lhsT, rhs], writes=[out], inc=inc)

    def transpose(self, out, in_, ident, inc=True):
        return self.op("pe", lambda t: t.transpose(_ap(out), _ap(in_), _ap(ident)),
                       reads=[in_, ident], writes=[out], inc=inc)

    def act(self, out, in_, func, bias=None, scale=None, accum=None, e="act"):
        kw = {}
        rd = [in_]
        wr = [out]
        if bias is not None:
            kw["bias"] = _ap(bias)
            rd.append(bias)
        if scale is not None:
            kw["scale"] = _ap(scale)
            rd.append(scale)
        if accum is not None:
            kw["accum_out"] = _ap(accum)
            wr.append(accum)
        return self.op(e, lambda a: a.activation(_ap(out), _ap(in_), func, **kw), reads=rd, writes=wr)

    def tt(self, out, a, b, op, e="dve"):
        return self.op(e, lambda v: v.tensor_tensor(_ap(out), _ap(a), _ap(b), op), reads=[a, b], writes=[out])

    def ts(self, out, a, s1, op0, s2=None, op1=None, accum=None, e="dve"):
        rd = [a, s1, s2]
        wr = [out]
        kw = {}
        if op1 is not None:
            kw["op1"] = op1
        if accum is not None:
            kw["accum_out"] = _ap(accum)
            wr.append(accum)
        return self.op(e, lambda v: v.tensor_scalar(_ap(out), _ap(a), _ap(s1), _ap(s2), op0, **kw), reads=rd, writes=wr)

    def stt(self, out, a, s, b, op0, op1, e="dve"):
        return self.op(e, lambda v: v.scalar_tensor_tensor(_ap(out), _ap(a), _ap(s), _ap(b), op0, op1),
                       reads=[a, s, b], writes=[out])

    def copy(self, out, in_, e="dve"):
        if e == "act":
            return self.op(e, lambda a: a.copy(_ap(out), _ap(in_)), reads=[in_], writes=[out])
        return self.op(e, lambda v: v.tensor_copy(_ap(out), _ap(in_)), reads=[in_], writes=[out])

    def memset(self, out, val, e="dve"):
        return self.op(e, lambda v: v.memset(_ap(out), val), reads=[], writes=[out])

    def reduce(self, out, in_, op, axis=AX.X, e="dve"):
        return self.op(e, lambda v: v.tensor_reduce(_ap(out), _ap(in_), axis, op), reads=[in_], writes=[out])

    def finish(self, out_tokens_engine="sp"):
        self.barrier()


from concourse.bass_utils import run_bass_kernel_spmd

D = 1024
PIN = 5416
FFN = 3584
NEXP = 8
O_QKV, O_Z, O_B, O_A, O_NQ, O_CK, O_CV, O_SK, O_SV, O_WK, O_WV, O_NG, O_MG = (
    0, 1536, 2048, 2056, 2064, 2576, 2704, 2832, 2960, 3088, 3216, 3344, 3368)
EPS = 1e-6
TWO_PI = 2.0 * np.pi


class Ctx:
    pass


class Pool:
    def __init__(self, P, name, n, shape, dt=F32):
        self.t = [P.sb("%s_%d" % (name, i), shape, dt) for i in range(n)]
        self.k = ["%s_%d" % (name, i) for i in range(n)]
        self.i = 0

    def next(self):
        j = self.i % len(self.t)
        self.i += 1
        return self.t[j], self.k[j]


def rsqrt_(P, out, in_, scale, bias_t):
    P.act(out, in_, AF.Sqrt, bias=bias_t, scale=scale)
    P.op("dve", lambda v: v.reciprocal(_ap(out), _ap(out)), reads=[out], writes=[out])


def stage0(P, C):
    nc = P.nc
    L, S = C.L, C.S
    with contextlib.ExitStack() as es:
        P.es, old = es, P.es
        cT = P.sb("s0_cT", [128, 8])
        cs = P.sb("s0_cs", [128, 8])
        wch = [P.sb("s0_w%d" % i, [128, 8, 512]) for i in range(2)]
        brow = P.sb("s0_b", [1, 6144])
        orow = P.sb("s0_o", [1, 6144])
        P.dma(V(cT[:, :], "cT"), C.c.rearrange("(c p) -> p c", p=128))
        P.act(V(cs[:, :], "cs"), V(cT[:, :], "cT"), AF.Silu)
        for l in range(L):
            P.dma(V(brow[:, :], "brow"), C.b_ada[l:l + 1, :])
            for j in range(12):
                w = wch[j % 2]
                wk = "s0w%d" % (j % 2)
                P.dma(V(w[:, :, :], wk), C.w_ada[l, :, j * 512:(j + 1) * 512].rearrange("(c p) n -> p c n", p=128),
                      q=("sp" if j % 2 == 0 else "act"))
                pb = C.psb[j % 2]
                pk = "psb%d" % (j % 2)
                for k in range(8):
                    P.mm(V(pb[0:1, :], pk), V(cs[:, k:k + 1], "cs"), V(w[:, k, :], wk), start=(k == 0), stop=(k == 7))
                P.tt(V(orow[:, j * 512:(j + 1) * 512], "orow"), V(pb[0:1, :], pk), V(brow[:, j * 512:(j + 1) * 512], "brow"), ALU.add)
            P.dma(V(C.mod_d[l:l + 1, :], "mod_d"), V(orow[:, :], "orow"))
        NT = S // 128
        posi = P.sb("s0_posi", [128, NT], I32)
        posf = P.sb("s0_posf", [128, NT])
        invf = P.sb("s0_invf", [128, 32])
        P.dma(V(posi[:, :], "posi"), C.pos.rearrange("(n p) -> p n", p=128))
        P.dma(V(invf[:, :], "invf"), C.invf)
        P.copy(V(posf[:, :], "posf"), V(posi[:, :], "posi"))
        up = Pool(P, "s0_u", 2, [128, 64])
        uip = Pool(P, "s0_ui", 2, [128, 64], I32)
        ufp = Pool(P, "s0_uf", 2, [128, 64])
        obp = Pool(P, "s0_ob", 2, [128, 64])
        for tt in range(NT):
            u, ku = up.next()
            ui, kui = uip.next()
            uf, kuf = ufp.next()
            ob, kob = obp.next()
            P.ts(V(u[:, 32:64], ku), V(invf[:, :], "invf"), V(posf[:, tt:tt + 1], "posf"), ALU.mult)
            P.ts(V(u[:, 0:32], ku), V(u[:, 32:64], ku), 0.25, ALU.add)
            P.copy(V(ui[:, :], kui), V(u[:, :], ku))
            P.copy(V(uf[:, :], kuf), V(ui[:, :], kui))
            P.tt(V(u[:, :], ku), V(u[:, :], ku), V(uf[:, :], kuf), ALU.subtract)
            P.act(V(ob[:, :], kob), V(u[:, :], ku), AF.Sin, scale=TWO_PI)
            P.dma(V(C.cs_d[tt * 128:(tt + 1) * 128, :], "cs_d%d" % tt), V(ob[:, :], kob))
        P.barrier()
        P.es = old


def load_layer_vecs(P, C, l, which):
    i0 = 0 if which == "m" else 3
    nwd = C.norm_mix if which == "m" else C.norm_ffn
    nw = P.sb("lv_nw", [128, 8])
    nsc = P.sb("lv_nsc", [128, 8])
    nsh = P.sb("lv_nsh", [128, 8])
    grow = P.sb("lv_g", [128, D])
    P.dma(V(nw[:, :], "lv_nw"), nwd[l, :].rearrange("(c p) -> p c", p=128))
    P.dma(V(nsh[:, :], "lv_nsh"), V(C.mod_d[l, (i0 + 0) * D:(i0 + 1) * D].rearrange("(c p) -> p c", p=128), "mod_d"))
    P.dma(V(nsc[:, :], "lv_nsc"), V(C.mod_d[l, (i0 + 1) * D:(i0 + 2) * D].rearrange("(c p) -> p c", p=128), "mod_d"))
    P.dma(V(grow[:, :], "lv_g"), V(C.mod_d[l:l + 1, (i0 + 2) * D:(i0 + 3) * D].partition_broadcast(128), "mod_d"))
    P.stt(V(nsc[:, :], "lv_nsc"), V(nsc[:, :], "lv_nsc"), 1.0, V(nw[:, :], "lv_nw"), ALU.add, ALU.mult)
    return V(nsc[:, :], "lv_nsc"), V(nsh[:, :], "lv_nsh"), V(grow[:, :], "lv_g")


def norm_mod_T(P, C, xt, kx, nsc, nsh, hT, khT, tok0=0, hT32=None):
    ss, kss = C.p_ss.next()
    sq, ksq = C.p_sq.next()
    P.act(V(sq[:, :], ksq), V(xt[:, :], kx), AF.Square, accum=V(ss[:, :], kss))
    rsqrt_(P, V(ss[:, :], kss), V(ss[:, :], kss), 1.0 / D, C.eps_t)
    P.ts(V(sq[:, :], ksq), V(xt[:, :], kx), V(ss[:, 0:1], kss), ALU.mult)
    for half in range(2):
        pb, pk = C.psum()
        for c4 in range(4):
            c = half * 4 + c4
            P.transpose(V(pb[:, c4 * 128:(c4 + 1) * 128], pk), V(sq[:, c * 128:(c + 1) * 128], ksq), C.ident, inc=(c4 == 3))
        for c4 in range(4):
            c = half * 4 + c4
            P.act(V(hT[:, c, tok0:tok0 + 128], khT), V(pb[:, c4 * 128:(c4 + 1) * 128], pk), AF.Identity,
                  bias=V(_ap(nsh)[:, c:c + 1], nsh.key), scale=V(_ap(nsc)[:, c:c + 1], nsc.key))
            if hT32 is not None:
                P.act(V(hT32[0][:, c, :], hT32[1]), V(pb[:, c4 * 128:(c4 + 1) * 128], pk), AF.Identity,
                      bias=V(_ap(nsh)[:, c:c + 1], nsh.key), scale=V(_ap(nsc)[:, c:c + 1], nsc.key))


def stage1(P, C, l):
    S = C.S
    with contextlib.ExitStack() as es:
        P.es, old = es, P.es
        nsc, nsh, grow = load_layer_vecs(P, C, l, "m")
        W = P.sb("s1_W", [128, 8, PIN], BF16)
        ncol = [(j * 512, min(512, PIN - j * 512)) for j in range((PIN + 511) // 512)]
        for (c0, cw) in ncol:
            P.dma(V(W[:, :, c0:c0 + cw], "s1W%d" % c0), C.w_in[l, :, c0:c0 + cw].rearrange("(c p) n -> p c n", p=128), q="pool")
        xp = Pool(P, "s1_x", 2, [128, D])
        hp = Pool(P, "s1_h", 2, [128, 8, 128], BF16)
        op_ = Pool(P, "s1_o", 2, [128, PIN])
        C.p_ss = Pool(P, "s1_ss", 2, [128, 1])
        C.p_sq = Pool(P, "s1_sq", 2, [128, D])
        for tt in range(S // 128):
            xt, kx = xp.next()
            hT, khT = hp.next()
            ot, ko = op_.next()
            P.dma(V(xt[:, :], kx), V(C.xcur[tt * 128:(tt + 1) * 128, :], "xcur%d" % tt))
            norm_mod_T(P, C, xt, kx, nsc, nsh, hT, khT)
            for j, (c0, cw) in enumerate(ncol):
                pb, pk = C.psum()
                for k in range(8):
                    P.mm(V(pb[:, 0:cw], pk), V(hT[:, k, :], khT), V(W[:, k, c0:c0 + cw], "s1W%d" % c0), start=(k == 0), stop=(k == 7))
                P.copy(V(ot[:, c0:c0 + cw], ko), V(pb[:, 0:cw], pk), e=("act" if j % 2 == 0 else "dve"))
            P.dma(V(C.proj_d[tt * 128:(tt + 1) * 128, :], "proj%d" % tt), V(ot[:, :], ko), q="act")
        P.barrier()
        P.es = old


def build_program(S, L, dbg=()):
    nc = bass.Bass("TRN2", target_bir_lowering=False)
    C = Ctx()
    C.S, C.L = S, L
    import os
    C.cut = int(os.environ.get('CUT', '0'))
    C.skip = os.environ.get('SKIP', '').split(',')
    din = lambda name, shape, dt=F32: nc.dram_tensor(name, list(shape), dt, kind="ExternalInput").ap()
    C.x = din("x", [S, D])
    C.c = din("c", [D])
    C.pos = din("positions", [S], I32)
    C.w_ada = din("w_ada", [L, D, 6 * D])
    C.b_ada = din("b_ada", [L, 6 * D])
    C.norm_mix = din("norm_mix", [L, D])
    C.norm_ffn = din("norm_ffn", [L, D])
    C.w_in = din("w_in", [L, D, PIN])
    C.conv_w = din("conv_w", [L, 4, 1536])
    C.a_log = din("a_log", [L, 8])
    C.dt_bias = din("dt_bias", [L, 8])
    C.dn_norm = din("dn_norm", [L, 64])
    C.cmp_pos = din("cmp_pos", [L, 2, 32, 64])
    C.w_cmp1 = din("w_cmp1", [L, 2, 2048, 64])
    C.w_cmp2 = din("w_cmp2", [L, 2, 64, 64])
    C.q_norm = din("q_norm", [L, 64])
    C.k_norm = din("k_norm", [L, 3, 64])
    C.w_oa = din("w_oa", [L, 512, D])
    C.w_ob = din("w_ob", [L, 512, D])
    C.w_out = din("w_out", [L, D, D])
    nd, nm = (L + 1) // 2, max(L // 2, 1)
    C.w1_dense = din("w1_dense", [nd, D, FFN])
    C.w3_dense = din("w3_dense", [nd, D, FFN])
    C.w2_dense = din("w2_dense", [nd, FFN, D])
    C.w_router = din("w_router", [nm, D, NEXP])
    C.w1_moe = din("w1_moe", [nm, NEXP, D, FFN])
    C.w3_moe = din("w3_moe", [nm, NEXP, D, FFN])
    C.w2_moe = din("w2_moe", [nm, NEXP, FFN, D])
    C.ident_d = din("k_ident", [128, 128])
    C.invf = din("k_invf", [128, 32])
    C.k_U = din("k_U", [128, 128])
    C.k_NM1s = din("k_NM1s", [128, 128])
    C.k_M2 = din("k_M2", [128, 128])
    C.k_SU = din("k_SU", [128, 128])
    NTc = S // 128
    C.k_Ov = din("k_Ov", [128, 4, 128])
    C.k_Ex = din("k_Ex", [128, NTc, 128])
    C.k_Mc = din("k_Mc", [128, 2304 + 128])
    C.k_Km = din("k_Km", [128, 256])
    C.k_Fm = din("k_Fm", [128, 256])
    C.out = nc.dram_tensor("out", [S, D], F32, kind="ExternalOutput").ap()
    dk = lambda name: ("ExternalOutput" if name in dbg else "Internal")
    dscr = lambda name, shape, dt=F32: nc.dram_tensor(name, list(shape), dt, kind=dk(name)).ap()
    C.mod_d = dscr("mod_d", [L, 6 * D])
    C.cs_d = dscr("cs_d", [S, 64])
    C.proj_d = dscr("proj_d", [S, PIN])
    C.oa_d = dscr("oa_d", [S, 512])
    C.ob_d = dscr("ob_d", [S, 512])
    C.xmid_d = dscr("xmid_d", [S, D])
    C.xl_d = dscr("xl_d", [S, D])
    C.qT_d = dscr("qT_d", [8, 64, S], BF16)
    C.kT_d = dscr("kT_d", [4, 64, S], BF16)
    C.xcur = C.x
    with contextlib.ExitStack() as es:
        es.enter_context(nc.allow_non_contiguous_dma(reason="small strided parameter loads"))
        P = Prog(nc, es)
        P.init()
        C.P = P
        C.psb = [P.ps("psb%d" % i, [128, 512]) for i in range(8)]
        C.psi = 0

        C.psn = 8

        def psum():
            j = C.psi % C.psn
            C.psi += 1
            return C.psb[j], "psb%d" % j
        C.psum = psum
        ident = P.sb("ident", [128, 128])
        eps_t = P.sb("eps_t", [128, 1])
        P.dma(V(ident[:, :], "ident"), C.ident_d)
        P.memset(V(eps_t[:, :], "eps"), EPS)
        one_t = P.sb("one_t", [128, 1])
        P.memset(V(one_t[:, :], "one"), 1.0)
        C.one_t = one_t[:, :]
        P.barrier()
        C.ident = ident[:, :]
        C.eps_t = eps_t[:, :]
        stage0(P, C)
        for l in range(L):
            C.xcur = C.x if l == 0 else C.xl_d
            stage1(P, C, l)
            if "s2" not in C.skip:
                stage2(P, C, l)
            if "s3" not in C.skip:
                stage3(P, C, l)
            if "s4" not in C.skip:
                stage4(P, C, l)
                stage5(P, C, l, C.out if l == L - 1 else C.xl_d)
        P.finish()
        print("ninst", P.ninst)
    return nc


def host_consts(S=8192):
    NTc = S // 128
    cc = np.arange(128)[:, None]
    jj = np.arange(128)[None, :]
    Ov = np.zeros((128, 4, 128), np.float32)
    for ct in range(4):
        cg = ct * 128 + cc
        Ov[:, ct, :] = ((16 * cg < 64 * jj + 64) & (16 * cg + 32 > 64 * jj))
    Ex = np.zeros((128, NTc, 128), np.float32)
    for kt in range(NTc):
        Ex[:, kt, :] = (cc == 2 * kt + jj // 64)
    col = np.arange(2304 + 128)[None, :]
    Mc = (col >= 16 * cc + 31).astype(np.float32)
    r = np.arange(128)[:, None]
    jp = np.arange(256)[None, :] - 128
    cur = r // 64
    Km = (jp < cur - 1).astype(np.float32)
    Fm = np.where(jp > cur, -FORCE, np.where(jp == cur, FORCE, np.where(jp == cur - 1, FORCE + 64.0, 0.0))).astype(np.float32)
    nsa = {"k_Ov": Ov, "k_Ex": Ex, "k_Mc": Mc, "k_Km": np.ascontiguousarray(Km), "k_Fm": np.ascontiguousarray(Fm)}
    d = _host_consts0()
    d.update(nsa)
    return d


def _host_consts0():
    inv = 1.0 / (10000.0 ** (np.arange(0, 64, 2, dtype=np.float32) / 64.0))
    inv = (inv.astype(np.float64) / (2 * np.pi)).astype(np.float32)
    p = np.arange(128)[:, None]
    j = np.arange(128)[None, :]
    f = lambda m: np.ascontiguousarray(m.astype(np.float32))
    return {"k_U": f(p <= j), "k_NM1s": f(np.where(j >= p, -BIG, 0.0)), "k_M2": f(np.where(j < p, -BIG, 0.0)),
            "k_SU": f(j > p),
            "k_ident": np.eye(128, dtype=np.float32),
            "k_invf": np.ascontiguousarray(np.broadcast_to(inv[None, :], (128, 32))).astype(np.float32)}


BIG = 30000.0


def stage2(P, C, l):
    S = C.S
    NCH = S // 128
    with contextlib.ExitStack() as es:
        P.es, old = es, P.es
        sb = P.sb
        ident = C.ident
        cw = sb("d_cw", [128, 4, 12])
        negA = sb("d_negA", [128, 8])
        dtb = sb("d_dtb", [128, 8])
        dnw = sb("d_dnw", [128, 64])
        Um = sb("d_U", [128, 128])
        NM1s = sb("d_NM1s", [128, 128])
        M2 = sb("d_M2", [128, 128])
        SU = sb("d_SU", [128, 128])
        ones128 = sb("d_ones", [128, 128])
        P.memset(V(ones128[:, :], "ones128"), 1.0)
        for k in range(4):
            P.dma(V(cw[:, k, :], "cw"), C.conv_w[l, k, :].rearrange("(c p) -> p c", p=128))
        P.dma(V(negA[:, :], "negA"), C.a_log[l:l + 1, :].partition_broadcast(128))
        P.dma(V(dtb[:, :], "dtb"), C.dt_bias[l:l + 1, :].partition_broadcast(128))
        P.dma(V(dnw[:, :], "dnw"), C.dn_norm[l:l + 1, :].partition_broadcast(128))
        P.dma(V(Um[:, :], "U"), C.k_U)
        P.dma(V(NM1s[:, :], "NM1s"), C.k_NM1s)
        P.dma(V(M2[:, :], "M2"), C.k_M2)
        P.dma(V(SU[:, :], "SU"), C.k_SU)
        P.act(V(negA[:, :], "negA"), V(negA[:, :], "negA"), AF.Exp)
        P.ts(V(negA[:, :], "negA"), V(negA[:, :], "negA"), -1.0, ALU.mult)
        fT = sb("d_fT", [128, 12, 131])
        P.memset(V(fT[:, :, :], "fT"), 0.0)
        Sst = sb("d_S", [64, 8, 64])
        P.memset(V(Sst[:, :, :], "S"), 0.0)
        pt = sb("d_pt", [128, 2064])
        cv = sb("d_cv", [128, 12, 128])
        qkv = sb("d_qkv", [128, 1536])
        sq = sb("d_sq", [128, 1024])
        ssn = sb("d_ssn", [128, 16])
        qkT = sb("d_qkT", [64, 16, 128])
        sc8 = {n: sb("d_" + n, [128, 8]) for n in ("g", "beta", "nbeta", "gc", "ngc", "egc", "bege", "kdf", "gl", "t8")}
        grep_ = [sb("d_grep%d" % h, [128, 128]) for h in range(8)]
        tmp1 = [sb("d_tmp1_%d" % h, [128, 128]) for h in range(8)]
        tmp2 = [sb("d_tmp2_%d" % h, [128, 128]) for h in range(8)]
        decS = [sb("d_decS%d" % h, [128, 128]) for h in range(8)]
        decT = [sb("d_decT%d" % h, [128, 128]) for h in range(8)]
        AT = [sb("d_AT%d" % h, [128, 128]) for h in range(8)]
        Pm = [[sb("d_P%d_%d" % (h, i), [128, 128]) for i in range(2)] for h in range(8)]
        PTm = [[sb("d_PT%d_%d" % (h, i), [128, 128]) for i in range(2)] for h in range(8)]
        XT = [[sb("d_XT%d_%d" % (h, i), [128, 128]) for i in range(2)] for h in range(8)]
        vb = sb("d_vb", [128, 512])
        kbe = sb("d_kbe", [128, 512])
        kdec = sb("d_kdec", [128, 512])
        u = sb("d_u", [128, 512])
        wT = sb("d_wT", [64, 8, 128])
        vnew = sb("d_vnew", [128, 512])
        tq = sb("d_tq", [128, 512])
        o = sb("d_o", [128, 512])
        osq = sb("d_osq", [128, 512])
        zs = sb("d_zs", [128, 512])
        oo = sb("d_oo", [128, 512])
        s8 = lambda n: V(sc8[n][:, :], "sc_" + n)
        for n in range(NCH):
            P.dma(V(pt[:, :], "pt"), V(C.proj_d[n * 128:(n + 1) * 128, 0:2064], "proj%d" % n))
            P.copy(V(fT[:, :, 0:3], "fT"), V(fT[:, :, 128:131], "fT"), e="pool")
            for b in range(3):
                pb, pk = C.psum()
                for c4 in range(4):
                    c = b * 4 + c4
                    P.transpose(V(pb[:, c4 * 128:(c4 + 1) * 128], pk), V(pt[:, c * 128:(c + 1) * 128], "pt"), ident, inc=(c4 == 3))
                P.copy(V(fT[:, b * 4:(b + 1) * 4, 3:131], "fT"), V(pb[:, :].rearrange("p (c t) -> p c t", c=4), pk),
                       e=("act" if b % 2 == 0 else "dve"))
            for c in range(12):
                b = c // 4
                e = "dve"
                P.ts(V(cv[:, c, :], "cvg%d" % b), V(fT[:, c, 0:128], "fT"), V(cw[:, 0, c:c + 1], "cw"), ALU.mult, e=e)
                for k in range(1, 4):
                    P.stt(V(cv[:, c, :], "cvg%d" % b), V(fT[:, c, k:k + 128], "fT"), V(cw[:, k, c:c + 1], "cw"),
                          V(cv[:, c, :], "cvg%d" % b), ALU.mult, ALU.add, e=e)
            for b in range(3):
                P.act(V(cv[:, b * 4:(b + 1) * 4, :], "cvg%d" % b), V(cv[:, b * 4:(b + 1) * 4, :], "cvg%d" % b), AF.Silu)
            for b in range(3):
                pb, pk = C.psum()
                for c4 in range(4):
                    c = b * 4 + c4
                    P.transpose(V(pb[:, c4 * 128:(c4 + 1) * 128], pk), V(cv[:, c, :], "cvg%d" % b), ident, inc=(c4 == 3))
                P.copy(V(qkv[:, b * 512:(b + 1) * 512], "qkv"), V(pb[:, :], pk), e=("act" if b % 2 == 1 else "dve"))
            if C.cut == 1:
                continue
            P.tt(V(sq[:, :], "sq"), V(qkv[:, 0:1024], "qkv"), V(qkv[:, 0:1024], "qkv"), ALU.mult)
            P.reduce(V(ssn[:, :], "ssn"), V(sq[:, :].rearrange("p (h d) -> p h d", d=64), "sq"), ALU.add)
            rsqrt_(P, V(ssn[:, :], "ssn"), V(ssn[:, :], "ssn"), 1.0, C.eps_t)
            P.ts(V(ssn[:, 0:8], "ssn"), V(ssn[:, 0:8], "ssn"), 0.125, ALU.mult)
            P.tt(V(qkv[:, 0:1024].rearrange("p (h d) -> p h d", d=64), "qkv"),
                 V(qkv[:, 0:1024].rearrange("p (h d) -> p h d", d=64), "qkv"),
                 V(ssn[:, :].unsqueeze(2).to_broadcast([128, 16, 64]), "ssn"), ALU.mult)
            if C.cut == 11:
                continue
            for b in range(4):
                pb, pk = C.psum()
                for c4 in range(4):
                    hh = b * 4 + c4
                    P.mm(V(pb[0:64, c4 * 128:(c4 + 1) * 128], pk), V(qkv[:, hh * 64:(hh + 1) * 64], "qkv"), ident, inc=(c4 == 3))
                P.copy(V(qkT[:, b * 4:(b + 1) * 4, :], "qkT"), V(pb[0:64, :].rearrange("p (c t) -> p c t", c=4), pk),
                       e=("act" if b % 2 == 0 else "dve"))
            if C.cut == 2:
                continue
            P.tt(s8("t8"), V(pt[:, O_A:O_A + 8], "pt"), V(dtb[:, :], "dtb"), ALU.add)
            P.act(s8("t8"), s8("t8"), AF.Exp)
            P.act(s8("t8"), s8("t8"), AF.Ln, bias=C.one_t)
            if C.cut == 31:
                continue
            P.tt(s8("g"), s8("t8"), V(negA[:, :], "negA"), ALU.mult)
            P.act(s8("beta"), V(pt[:, O_B:O_B + 8], "pt"), AF.Sigmoid)
            P.ts(s8("nbeta"), s8("beta"), -1.0, ALU.mult)
            if C.cut == 32:
                continue
            pb, pk = C.psum()
            P.mm(V(pb[:, 0:8], pk), V(Um[:, :], "U"), s8("g"))
            P.copy(s8("gc"), V(pb[:, 0:8], pk))
            if C.cut == 33:
                continue
            P.ts(s8("ngc"), s8("gc"), -1.0, ALU.mult)
            P.act(s8("egc"), s8("gc"), AF.Exp)
            P.tt(s8("bege"), s8("beta"), s8("egc"), ALU.mult)
            if C.cut == 3:
                continue
            for h in range(8):
                kh = "h%d" % h
                P.ts(V(grep_[h][:, :], "grep" + kh), V(ones128[:, :], "ones128"), V(sc8["g"][:, h:h + 1], "sc_g"), ALU.mult)
                if 41 <= C.cut <= 41:
                    continue
                pg, pgk = C.psum()
                P.mm(V(pg[:, 0:128], pgk), V(grep_[h][:, :], "grep" + kh), V(Um[:, :], "U"))
                if 41 <= C.cut <= 42:
                    continue
                P.stt(V(tmp1[h][:, :], "tmp1" + kh), V(pg[:, 0:128], pgk), -1.0, V(NM1s[:, :], "NM1s"), ALU.mult, ALU.add)
                P.tt(V(tmp2[h][:, :], "tmp2" + kh), V(pg[:, 0:128], pgk), V(M2[:, :], "M2"), ALU.add)
                if 41 <= C.cut <= 43:
                    continue
                P.act(V(sc8["gl"][:, h:h + 1], "sc_gl"), V(tmp2[h][:, 127:128], "tmp2" + kh), AF.Exp)
                if 41 <= C.cut <= 44:
                    continue
                P.act(V(decS[h][:, :], "decS" + kh), V(tmp1[h][:, :], "tmp1" + kh), AF.Exp, bias=V(sc8["gc"][:, h:h + 1], "sc_gc"))
                P.act(V(decT[h][:, :], "decT" + kh), V(tmp2[h][:, :], "tmp2" + kh), AF.Exp, bias=V(sc8["ngc"][:, h:h + 1], "sc_ngc"))
                if 41 <= C.cut <= 45:
                    continue
                P.copy(V(sc8["kdf"][:, h:h + 1], "sc_kdf"), V(decT[h][:, 127:128], "decT" + kh))
                if 41 <= C.cut <= 46:
                    continue
                pm, pmk = C.psum()
                P.mm(V(pm[:, 0:128], pmk), V(qkT[:, 8 + h, :], "qkT"), V(qkT[:, 8 + h, :], "qkT"), inc=False)
                P.mm(V(pm[:, 128:256], pmk), V(qkT[:, 8 + h, :], "qkT"), V(qkT[:, h, :], "qkT"))
                if 41 <= C.cut <= 47:
                    continue
                P.stt(V(Pm[h][0][:, :], "P0" + kh), V(pm[:, 0:128], pmk), V(sc8["nbeta"][:, h:h + 1], "sc_nbeta"),
                      V(decS[h][:, :], "decS" + kh), ALU.mult, ALU.mult)
                if 41 <= C.cut <= 48:
                    continue
                P.tt(V(AT[h][:, :], "AT" + kh), V(pm[:, 128:256], pmk), V(decT[h][:, :], "decT" + kh), ALU.mult)
                if 41 <= C.cut <= 49:
                    continue
                P.mm(V(pm[:, 256:384], pmk), V(Pm[h][0][:, :], "P0" + kh), ident)
                P.copy(V(PTm[h][0][:, :], "PT0" + kh), V(pm[:, 256:384], pmk), e="dve")
                P.tt(V(XT[h][0][:, :], "XT0" + kh), V(pm[:, 256:384], pmk), ident, ALU.add)
            if C.cut == 4 or C.cut >= 40:
                continue
            q3 = lambda t: t[:, :].rearrange("p (h d) -> p h d", d=64)
            bc = lambda n: V(sc8[n][:, :].unsqueeze(2).to_broadcast([128, 8, 64]), "sc_" + n)
            P.tt(V(q3(vb), "vb"), V(qkv[:, 1024:1536].rearrange("p (h d) -> p h d", d=64), "qkv"), bc("beta"), ALU.mult)
            P.tt(V(q3(kbe), "kbe"), V(qkv[:, 512:1024].rearrange("p (h d) -> p h d", d=64), "qkv"), bc("bege"), ALU.mult)
            P.tt(V(q3(kdec), "kdec"), V(qkv[:, 512:1024].rearrange("p (h d) -> p h d", d=64), "qkv"), bc("kdf"), ALU.mult)
            if C.cut == 5:
                continue
            for lev in range(6):
                a, b2 = lev % 2, (lev + 1) % 2
                for h in range(8):
                    kh = "h%d" % h
                    kP, kPT, kX = "P%d" % a + kh, "PT%d" % a + kh, "XT%d" % a + kh
                    nP, nPT, nX = "P%d" % b2 + kh, "PT%d" % b2 + kh, "XT%d" % b2 + kh
                    pm, pmk = C.psum()
                    P.mm(V(pm[:, 0:128], pmk), V(PTm[h][a][:, :], kPT), V(Pm[h][a][:, :], kP), inc=(lev == 5))
                    if lev < 5:
                        P.mm(V(pm[:, 128:256], pmk), V(Pm[h][a][:, :], kP), V(PTm[h][a][:, :], kPT))
                    P.copy(V(Pm[h][b2][:, :], nP), V(pm[:, 0:128], pmk), e="act")
                    if lev < 5:
                        P.copy(V(PTm[h][b2][:, :], nPT), V(pm[:, 128:256], pmk), e="act")
                    px, pxk = C.psum()
                    P.mm(V(px[:, 0:128], pxk), V(Pm[h][b2][:, :], nP), V(XT[h][a][:, :], kX))
                    P.tt(V(XT[h][b2][:, :], nX), V(px[:, 0:128], pxk), V(XT[h][a][:, :], kX), ALU.add)
            if C.cut == 6:
                continue
            pu, puk = C.psum()
            pw, pwk = C.psum()
            for h in range(8):
                kX = "XT0h%d" % h
                P.mm(V(pu[:, h * 64:(h + 1) * 64], puk), V(XT[h][0][:, :], kX), V(vb[:, h * 64:(h + 1) * 64], "vb"), inc=(h == 7))
            for h in range(8):
                kX = "XT0h%d" % h
                if h == 4:
                    pw2, pw2k = C.psum()
                pwb, pwbk = (pw, pwk) if h < 4 else (pw2, pw2k)
                P.mm(V(pwb[0:64, (h % 4) * 128:(h % 4 + 1) * 128], pwbk), V(kbe[:, h * 64:(h + 1) * 64], "kbe"), V(XT[h][0][:, :], kX), inc=(h % 4 == 3))
            P.copy(V(u[:, :], "u"), V(pu[:, :], puk), e="act")
            P.copy(V(wT[:, 0:4, :], "wT"), V(pw[0:64, :].rearrange("p (c t) -> p c t", c=4), pwk), e="dve")
            P.copy(V(wT[:, 4:8, :], "wT"), V(pw2[0:64, :].rearrange("p (c t) -> p c t", c=4), pw2k), e="act")
            if C.cut == 7:
                continue
            p1, p1k = C.psum()
            p2, p2k = C.psum()
            for h in range(8):
                P.mm(V(p1[:, h * 64:(h + 1) * 64], p1k), V(wT[:, h, :], "wT"), V(Sst[:, h, :], "S"), inc=(h == 7))
            for h in range(8):
                P.mm(V(p2[:, h * 64:(h + 1) * 64], p2k), V(qkT[:, h, :], "qkT"), V(Sst[:, h, :], "S"), inc=(h == 7))
            P.tt(V(vnew[:, :], "vnew"), V(u[:, :], "u"), V(p1[:, :], p1k), ALU.subtract)
            P.tt(V(q3(tq), "tq"), V(p2[:, :].rearrange("p (h d) -> p h d", d=64), p2k), bc("egc"), ALU.mult)
            p3, p3k = C.psum()
            p4, p4k = C.psum()
            for h in range(8):
                P.mm(V(p3[:, h * 64:(h + 1) * 64], p3k), V(AT[h][:, :], "ATh%d" % h), V(vnew[:, h * 64:(h + 1) * 64], "vnew"), inc=(h == 7))
            for h in range(8):
                P.mm(V(p4[0:64, h * 64:(h + 1) * 64], p4k), V(kdec[:, h * 64:(h + 1) * 64], "kdec"), V(vnew[:, h * 64:(h + 1) * 64], "vnew"), inc=(h == 7))
            P.tt(V(o[:, :], "o"), V(p3[:, :], p3k), V(tq[:, :], "tq"), ALU.add)
            P.tt(V(Sst[:, :, :], "S"), V(Sst[:, :, :], "S"), V(sc8["gl"][0:64, :].unsqueeze(2).to_broadcast([64, 8, 64]), "sc_gl"), ALU.mult)
            P.tt(V(Sst[:, :, :], "S"), V(Sst[:, :, :], "S"), V(p4[0:64, :].rearrange("p (h d) -> p h d", d=64), p4k), ALU.add)
            if C.cut == 8:
                continue
            P.tt(V(osq[:, :], "osq"), V(o[:, :], "o"), V(o[:, :], "o"), ALU.mult, e="pool")
            P.reduce(s8("t8"), V(q3(osq), "osq"), ALU.add)
            rsqrt_(P, s8("t8"), s8("t8"), 1.0 / 64, C.eps_t)
            P.tt(V(q3(o), "o"), V(q3(o), "o"), bc("t8"), ALU.mult)
            P.tt(V(q3(o), "o"), V(q3(o), "o"), V(dnw[:, :].unsqueeze(1).to_broadcast([128, 8, 64]), "dnw"), ALU.mult)
            P.act(V(zs[:, :], "zs"), V(pt[:, O_Z:O_Z + 512], "pt"), AF.Silu)
            P.tt(V(oo[:, :], "oo"), V(o[:, :], "o"), V(zs[:, :], "zs"), ALU.mult)
            P.dma(V(C.oa_d[n * 128:(n + 1) * 128, :], "oa%d" % n), V(oo[:, :], "oo"), q="act")
        P.barrier()
        P.es = old


FORCE = 1.0e6
O_N0 = O_NQ
N_NSA = O_MG - O_NQ


def stage3(P, C, l):
    S = C.S
    NT = S // 128
    NCMP = (S - 32) // 16 + 1
    NCT = (NCMP + 127) // 128
    ident = C.ident
    with contextlib.ExitStack() as es0:
        P.es, old = es0, P.es
        sb = P.sb
        identb = sb("n_identb", [128, 128], BF16)
        P.copy(V(identb[:, :], "identb"), ident)
        svp = sb("n_svp", [128, NT, 2, 65], BF16)
        wvp = sb("n_wvp", [128, NT, 2, 65], BF16)
        gates = sb("n_gates", [128, NT, 24])
        rhsc = sb("n_rhsc", [128, NCT, 2, 193], BF16)
        kcT = sb("n_kcT", [64, 2, NCT * 128], BF16)
        P.memset(V(svp[:, :, :, 64:65], "svp"), 1.0)
        P.memset(V(wvp[:, :, :, 64:65], "wvp"), 1.0)
        P.memset(V(rhsc[:, :, :, :], "rhsc"), 0.0)
        P.memset(V(kcT[:, :, :], "kcT"), 0.0)
        P.memset(V(rhsc[:, :, :, 64:65], "rhsc"), 1.0)
        ovt = sb("n_ovt", [128, NCT, 128])
        P.dma(V(ovt[:, :, :], "ovt"), C.k_Ov[:, 0:NCT, :])
        for g in range(2):
            P.copy(V(rhsc[:, :, g, 65:193], "rhsc"), V(ovt[:, :, :], "ovt"))
        with contextlib.ExitStack() as es1:
            P.es = es1
            ccT = sb("n_ccT", [64, 4, S], BF16)
            w20 = sb("n_w20", [128, 20, 64])
            P.memset(V(w20[:, :, :], "w20"), 1.0)
            for h in range(8):
                P.dma(V(w20[:, h, :], "w20"), C.q_norm[l:l + 1, :].partition_broadcast(128))
            for j, hh in ((1, 12), (1, 13), (2, 16), (2, 17)):
                P.dma(V(w20[:, hh, :], "w20"), C.k_norm[l, j:j + 1, :].partition_broadcast(128))
            P.ts(V(w20[:, 0:8, :], "w20"), V(w20[:, 0:8, :], "w20"), 0.125, ALU.mult)
            pt = sb("n_pt", [128, N_NSA])
            cst = sb("n_cs", [128, 64])
            sq = sb("n_sq", [128, 1280])
            ss = sb("n_ss", [128, 20])
            xn = sb("n_xn", [128, 20, 64])
            ro = sb("n_ro", [128, 20, 64])
            t1 = sb("n_t1", [128, 20, 32])
            rob = sb("n_rob", [128, 20, 64], BF16)
            ptb = sb("n_ptb", [128, 1280], BF16)
            qTt = sb("n_qTt", [64, 8, 128], BF16)
            kTt = sb("n_kTt", [64, 4, 128], BF16)
            for tt in range(NT):
                P.dma(V(pt[:, :], "pt"), V(C.proj_d[tt * 128:(tt + 1) * 128, O_N0:O_N0 + N_NSA], "proj%d" % tt))
                P.dma(V(cst[:, :], "cst"), V(C.cs_d[tt * 128:(tt + 1) * 128, :], "cs_d%d" % tt))
                x3 = pt[:, 0:1280].rearrange("p (h d) -> p h d", d=64)
                P.tt(V(sq[:, :], "sq"), V(pt[:, 0:1280], "pt"), V(pt[:, 0:1280], "pt"), ALU.mult)
                P.reduce(V(ss[:, :], "ss"), V(sq[:, :].rearrange("p (h d) -> p h d", d=64), "sq"), ALU.add)
                rsqrt_(P, V(ss[:, :], "ss"), V(ss[:, :], "ss"), 1.0 / 64, C.eps_t)
                P.tt(V(xn[:, :, :], "xn"), V(x3, "pt"), V(ss[:, :].unsqueeze(2).to_broadcast([128, 20, 64]), "ss"), ALU.mult)
                P.tt(V(xn[:, :, :], "xn"), V(xn[:, :, :], "xn"), V(w20[:, :, :], "w20"), ALU.mult)
                cosb = V(cst[:, 0:32].unsqueeze(1).to_broadcast([128, 20, 32]), "cst")
                sinb = V(cst[:, 32:64].unsqueeze(1).to_broadcast([128, 20, 32]), "cst")
                P.tt(V(ro[:, :, 0:32], "ro"), V(xn[:, :, 0:32], "xn"), cosb, ALU.mult)
                P.tt(V(t1[:, :, :], "t1"), V(xn[:, :, 32:64], "xn"), sinb, ALU.mult)
                P.tt(V(ro[:, :, 0:32], "ro"), V(ro[:, :, 0:32], "ro"), V(t1[:, :, :], "t1"), ALU.subtract)
                P.tt(V(ro[:, :, 32:64], "ro"), V(xn[:, :, 32:64], "xn"), cosb, ALU.mult)
                P.tt(V(t1[:, :, :], "t1"), V(xn[:, :, 0:32], "xn"), sinb, ALU.mult)
                P.tt(V(ro[:, :, 32:64], "ro"), V(ro[:, :, 32:64], "ro"), V(t1[:, :, :], "t1"), ALU.add)
                P.copy(V(rob[:, :, :], "rob"), V(ro[:, :, :], "ro"), e="act")
                P.copy(V(ptb[:, :], "ptb"), V(pt[:, 0:1280], "pt"), e="act")
                P.copy(V(svp[:, tt, :, 0:64], "svp"), V(pt[:, 896:1024].rearrange("p (g d) -> p g d", d=64), "pt"), e="pool")
                P.copy(V(wvp[:, tt, :, 0:64], "wvp"), V(pt[:, 1152:1280].rearrange("p (g d) -> p g d", d=64), "pt"), e="pool")
                P.act(V(gates[:, tt, :], "gates"), V(pt[:, 1280:1304], "pt"), AF.Sigmoid)
                for b in range(2):
                    pb, pk = C.psum()
                    for c4 in range(4):
                        hh = b * 4 + c4
                        P.mm(V(pb[0:64, c4 * 128:(c4 + 1) * 128], pk), V(rob[:, hh, :], "rob"), V(identb[:, :], "identb"), inc=(c4 == 3))
                    P.copy(V(qTt[:, b * 4:(b + 1) * 4, :], "qTt"), V(pb[0:64, :].rearrange("p (c t) -> p c t", c=4), pk), e="act")
                P.dma(V(C.qT_d[:, :, tt * 128:(tt + 1) * 128].rearrange("h d t -> d h t"), "qT_d%d" % tt), V(qTt[:, :, :], "qTt"), q="act")
                pb, pk = C.psum()
                for c4, hh in enumerate((12, 13, 16, 17)):
                    P.mm(V(pb[0:64, c4 * 128:(c4 + 1) * 128], pk), V(rob[:, hh, :], "rob"), V(identb[:, :], "identb"), inc=(c4 == 3))
                P.copy(V(kTt[:, :, :], "kTt"), V(pb[0:64, :].rearrange("p (c t) -> p c t", c=4), pk), e="dve")
                P.dma(V(C.kT_d[:, :, tt * 128:(tt + 1) * 128].rearrange("h d t -> d h t"), "kT_d%d" % tt), V(kTt[:, :, :], "kTt"), q="act")
                pb, pk = C.psum()
                for c4 in range(4):
                    P.mm(V(pb[0:64, c4 * 128:(c4 + 1) * 128], pk), V(ptb[:, 512 + c4 * 64:512 + (c4 + 1) * 64], "ptb"), V(identb[:, :], "identb"), inc=(c4 == 3))
                P.copy(V(ccT[:, :, tt * 128:(tt + 1) * 128], "ccT"), V(pb[0:64, :].rearrange("p (c t) -> p c t", c=4), pk), e="dve")
            w1 = sb("n_w1", [64, 32, 64], BF16)
            w1f = sb("n_w1f", [128, 16, 64])
            posf = sb("n_posf", [128, 16])
            w2 = sb("n_w2", [64, 64], BF16)
            bia = sb("n_bia", [64, 1])
            h1 = sb("n_h1", [64, NCT * 128], BF16)
            P.memset(V(h1[:, :], "h1"), 0.0)
            kw0 = sb("n_kw0", [128, 64])
            P.dma(V(kw0[:, :], "kw0"), C.k_norm[l, 0:1, :].partition_broadcast(128))
            csc = sb("n_csc", [128, NCT, 64])
            P.memset(V(csc[:, :, :], "csc"), 0.0)
            for ct in range(NCT):
                n = min(128, NCMP - ct * 128)
                r0 = 31 + 16 * ct * 128
                P.dma(V(csc[0:n, ct, :], "csc"), V(C.cs_d[r0:r0 + 16 * (n - 1) + 1:16, :], "cs_all"))
            kc = sb("n_kc", [128, 64])
            kc2 = sb("n_kc2", [128, 64])
            kss = sb("n_kss", [128, 1])
            kt1 = sb("n_kt1", [128, 32])
            kcb = sb("n_kcb", [128, 64], BF16)
            for j in range(2):
                for g in range(2):
                    src = ccT[:, j * 2 + g, :]
                    P.dma(V(w1[:, :, :], "w1"), C.w_cmp1[l, j].rearrange("(l d) o -> d l o", d=64), q="pool")
                    P.dma(V(w1f[:, :, :], "w1f"), C.w_cmp1[l, j].rearrange("(c p) o -> p c o", p=128))
                    P.dma(V(posf[:, :], "posf"), C.cmp_pos[l, j].rearrange("(c a) d -> (a d) c", a=2))
                    P.dma(V(w2[:, :], "w2"), C.w_cmp2[l, j], q="pool")
                    pbb, pbk = C.psum()
                    for c in range(16):
                        P.mm(V(pbb[0:64, 0:1], pbk), V(w1f[:, c, :], "w1f"), V(posf[:, c:c + 1], "posf"), start=(c == 0), stop=(c == 15))
                    P.copy(V(bia[:, :], "bia"), V(pbb[0:64, 0:1], pbk))
                    pb, pk = C.psum()
                    for ll in range(32):
                        P.mm(V(pb[0:64, 0:NCMP], pk), V(w1[:, ll, :], "w1"), V(src[:, ll:ll + 16 * (NCMP - 1) + 1:16], "ccT"),
                             start=(ll == 0), stop=(ll == 31))
                    P.act(V(h1[:, 0:NCMP], "h1"), V(pb[0:64, 0:NCMP], pk), AF.Silu, bias=V(bia[:, :], "bia"))
                    for ct in range(NCT):
                        po, pok = C.psum()
                        P.mm(V(po[:, 0:64], pok), V(h1[:, ct * 128:(ct + 1) * 128], "h1"), V(w2[:, :], "w2"))
                        if j == 1:
                            P.copy(V(rhsc[:, ct, g, 0:64], "rhsc"), V(po[:, 0:64], pok))
                        else:
                            P.copy(V(kc[:, :], "kc"), V(po[:, 0:64], pok))
                            P.tt(V(kc2[:, :], "kc2"), V(kc[:, :], "kc"), V(kc[:, :], "kc"), ALU.mult)
                            P.reduce(V(kss[:, :], "kss"), V(kc2[:, :], "kc2"), ALU.add)
                            rsqrt_(P, V(kss[:, :], "kss"), V(kss[:, :], "kss"), 1.0 / 64, C.eps_t)
                            P.stt(V(kc[:, :], "kc"), V(kc[:, :], "kc"), V(kss[:, 0:1], "kss"), V(kw0[:, :], "kw0"), ALU.mult, ALU.mult)
                            cc_, sc_ = V(csc[:, ct, 0:32], "csc"), V(csc[:, ct, 32:64], "csc")
                            P.tt(V(kc2[:, 0:32], "kc2"), V(kc[:, 0:32], "kc"), cc_, ALU.mult)
                            P.tt(V(kt1[:, :], "kt1"), V(kc[:, 32:64], "kc"), sc_, ALU.mult)
                            P.tt(V(kc2[:, 0:32], "kc2"), V(kc2[:, 0:32], "kc2"), V(kt1[:, :], "kt1"), ALU.subtract)
                            P.tt(V(kc2[:, 32:64], "kc2"), V(kc[:, 32:64], "kc"), cc_, ALU.mult)
                            P.tt(V(kt1[:, :], "kt1"), V(kc[:, 0:32], "kc"), sc_, ALU.mult)
                            P.tt(V(kc2[:, 32:64], "kc2"), V(kc2[:, 32:64], "kc2"), V(kt1[:, :], "kt1"), ALU.add)
                            P.copy(V(kcb[:, :], "kcb"), V(kc2[:, :], "kc2"))
                            pq, pqk = C.psum()
                            P.mm(V(pq[0:64, 0:128], pqk), V(kcb[:, :], "kcb"), V(identb[:, :], "identb"))
                            P.copy(V(kcT[:, g, ct * 128:(ct + 1) * 128], "kcT"), V(pq[0:64, 0:128], pqk))
            P.barrier()
        P.es = es0
        stage3b(P, C, l, svp, wvp, gates, rhsc, kcT, identb)
        P.barrier()
        P.es = old


def stage3b(P, C, l, svp, wvp, gates, rhsc, kcT, identb):
    S = C.S
    NT = S // 128
    NCMP = (S - 32) // 16 + 1
    NCT = (NCMP + 127) // 128
    with contextlib.ExitStack() as es:
        P.es = es
        sb = P.sb
        kTa = sb("b_kTa", [64, 4, S], BF16)
        for i in range(4):
            P.dma(V(kTa[:, i, :], "kTa"), V(C.kT_d[i], "kT_d_all"))
        Ex = sb("b_Ex", [128, NT, 128], BF16)
        Exf = sb("b_Exf", [128, 128])
        for kt in range(NT):
            P.dma(V(Exf[:, :], "Exf"), C.k_Ex[:, kt, :])
            P.copy(V(Ex[:, kt, :], "Ex"), V(Exf[:, :], "Exf"))
        cf = sb("b_cf", [128, 2304 + 128])
        Mc = sb("b_Mc", [128, 2304 + 128], BF16)
        P.dma(V(cf[:, :], "cf"), C.k_Mc)
        P.copy(V(Mc[:, :], "Mc"), V(cf[:, :], "cf"))
        Km = sb("b_Km", [128, 256])
        Fm = sb("b_Fm", [128, 256])
        P.dma(V(Km[:, :], "Km"), C.k_Km)
        P.dma(V(Fm[:, :], "Fm"), C.k_Fm)
        Dm = sb("b_D", [128, 128], BF16)
        Dn = sb("b_Dn", [128, 128], BF16)
        P.dma(V(cf[:, 0:128], "cf"), C.k_U)
        P.copy(V(Dm[:, :], "D"), V(cf[:, 0:128], "cf"))
        P.ts(V(cf[:, 0:128], "cf"), V(cf[:, 0:128], "cf"), -1.0, ALU.mult, 1.0, ALU.add)
        P.copy(V(Dn[:, :], "Dn"), V(cf[:, 0:128], "cf"))
        qTq = sb("b_qTq", [64, 8, 128], BF16)
        ecp = [sb("b_ec%d" % i, [128, 512], BF16) for i in range(NCT)]
        impacc = sb("b_imp", [128, 128])
        impadj = sb("b_impadj", [128, 128])
        imr = sb("b_imr", [128, 128])
        m8a = sb("b_m8a", [128, 8])
        m8b = sb("b_m8b", [128, 8])
        sel = sb("b_sel", [128, 128], BF16)
        selT = sb("b_selT", [128, 128], BF16)
        rz = sb("b_rz", [128, 1])
        gz = sb("b_gz", [128, 1])
        ob = sb("b_ob", [128, 8, 64])
        mskp = Pool(P, "b_msk", 2, [128, 128], BF16)
        ep = Pool(P, "b_e", 3, [128, 512], BF16)
        emp = Pool(P, "b_em", 3, [128, 512], BF16)
        v4 = lambda t: t[:, :].rearrange("p (h t) -> p h t", h=4)
        b4 = lambda a: a.unsqueeze(1).to_broadcast([128, 4, 128])

        def finish_head(pacc, pk_, hh, gi, first):
            c0 = 0
            P.ts(V(rz[:, :], "rz"), V(pacc[:, c0 + 64:c0 + 65], pk_), 1e-30, ALU.max)
            P.op("dve", lambda v: v.reciprocal(rz[:, :], rz[:, :]), reads=[V(rz[:, :], "rz")], writes=[V(rz[:, :], "rz")])
            P.tt(V(gz[:, :], "gz"), V(rz[:, :], "rz"), V(gates[:, C._qt, hh * 3 + gi:hh * 3 + gi + 1], "gates"), ALU.mult)
            if first:
                P.ts(V(ob[:, hh, :], "ob"), V(pacc[:, c0:c0 + 64], pk_), V(gz[:, 0:1], "gz"), ALU.mult)
            else:
                P.stt(V(ob[:, hh, :], "ob"), V(pacc[:, c0:c0 + 64], pk_), V(gz[:, 0:1], "gz"), V(ob[:, hh, :], "ob"), ALU.mult, ALU.add)

        C.psn = 4
        for qt in range(NT):
            C._qt = qt
            t0 = qt * 128
            P.dma(V(qTq[:, :, :], "qTq"), V(C.qT_d[:, :, t0:t0 + 128].rearrange("h d t -> d h t"), "qT_d_all"))
            for g in range(2):
                cts = [ct for ct in range(NCT) if t0 - 2048 * ct >= 0]
                for ct in cts:
                    o_ = t0 - 2048 * ct
                    ps, psk = C.psum()
                    for h in range(4):
                        P.mm(V(ps[:, h * 128:(h + 1) * 128], psk), V(kcT[:, g, ct * 128:(ct + 1) * 128], "kcT"), V(qTq[:, g * 4 + h, :], "qTq"), inc=(h == 3))
                    P.act(V(ecp[ct][:, :], "ec%d" % ct), V(ps[:, :], psk), AF.Exp)
                    if o_ < 2304:
                        P.tt(V(v4(ecp[ct]), "ec%d" % ct), V(v4(ecp[ct]), "ec%d" % ct), V(b4(Mc[:, o_:o_ + 128]), "Mc"), ALU.mult)
                for h in range(4):
                    hh = g * 4 + h
                    pc, pck = C.psum()
                    for i, ct in enumerate(cts):
                        P.mm(V(pc[:, 0:193], pck), V(ecp[ct][:, h * 128:(h + 1) * 128], "ec%d" % ct), V(rhsc[:, ct, g, :], "rhsc"),
                             start=(i == 0), stop=(i == len(cts) - 1))
                    pcv = V(pc[:, :], pck)
                    P.ts(V(rz[:, :], "rz"), V(pc[:, 64:65], pck), 1e-30, ALU.max)
                    P.op("dve", lambda v: v.reciprocal(rz[:, :], rz[:, :]), reads=[V(rz[:, :], "rz")], writes=[V(rz[:, :], "rz")])
                    if h == 0:
                        P.ts(V(impacc[:, :], "imp"), V(pc[:, 65:193], pck), V(rz[:, 0:1], "rz"), ALU.mult)
                    else:
                        P.stt(V(impacc[:, :], "imp"), V(pc[:, 65:193], pck), V(rz[:, 0:1], "rz"), V(impacc[:, :], "imp"), ALU.mult, ALU.add)
                    P.tt(V(gz[:, :], "gz"), V(rz[:, :], "rz"), V(gates[:, qt, hh * 3:hh * 3 + 1], "gates"), ALU.mult)
                    P.ts(V(ob[:, hh, :], "ob"), V(pc[:, 0:64], pck), V(gz[:, 0:1], "gz"), ALU.mult)
                off = 128 - 2 * qt
                P.tt(V(impadj[:, :], "impadj"), V(impacc[:, :], "imp"), V(Km[:, off:off + 128], "Km"), ALU.mult)
                P.tt(V(impadj[:, :], "impadj"), V(impadj[:, :], "impadj"), V(Fm[:, off:off + 128], "Fm"), ALU.add)
                P.memset(V(impadj[:, 0:1], "impadj"), FORCE + 128.0)
                P.op("dve", lambda v: v.max(m8a[:, :], impadj[:, :]), reads=[V(impadj[:, :], "impadj")], writes=[V(m8a[:, :], "m8a")])
                P.op("dve", lambda v: v.match_replace(imr[:, :], m8a[:, :], impadj[:, :], -3.0e6),
                     reads=[V(impadj[:, :], "impadj"), V(m8a[:, :], "m8a")], writes=[V(imr[:, :], "imr")])
                P.op("dve", lambda v: v.max(m8b[:, :], imr[:, :]), reads=[V(imr[:, :], "imr")], writes=[V(m8b[:, :], "m8b")])
                P.ts(V(sel[:, :], "sel"), V(impadj[:, :], "impadj"), V(m8b[:, 7:8], "m8b"), ALU.is_ge)
                pt_, ptk = C.psum()
                P.mm(V(pt_[:, 0:128], ptk), V(sel[:, :], "sel"), V(identb[:, :], "identb"))
                P.copy(V(selT[:, :], "selT"), V(pt_[:, 0:128], ptk))
                for br in range(2):
                    kbase = 0 if br == 0 else 2
                    vp = svp if br == 0 else wvp
                    vk = "svp" if br == 0 else "wvp"
                    kts = list(range(0, qt + 1)) if br == 0 else [kt for kt in range(qt - 4, qt + 1) if kt >= 0]
                    for i, kt in enumerate(kts):
                        mk = None
                        if br == 0:
                            pm, pmk = C.psum()
                            P.mm(V(pm[:, 0:128], pmk), V(Ex[:, kt, :], "Ex"), V(selT[:, :], "selT"))
                            msk, mskk = mskp.next()
                            P.copy(V(msk[:, :], mskk), V(pm[:, 0:128], pmk), e="act")
                            if kt == qt:
                                P.tt(V(msk[:, :], mskk), V(msk[:, :], mskk), V(Dm[:, :], "D"), ALU.mult, e="pool")
                            mk = V(msk[:, :], mskk)
                        else:
                            if kt == qt:
                                mk = V(Dm[:, :], "D")
                            elif kt == qt - 4:
                                mk = V(Dn[:, :], "Dn")
                        ps, psk = C.psum()
                        for h in range(4):
                            P.mm(V(ps[:, h * 128:(h + 1) * 128], psk), V(kTa[:, kbase + g, kt * 128:(kt + 1) * 128], "kTa"), V(qTq[:, g * 4 + h, :], "qTq"), inc=(h == 3))
                        e_, ek = ep.next()
                        P.act(V(e_[:, :], ek), V(ps[:, :], psk), AF.Exp)
                        if mk is not None:
                            em, emk = emp.next()
                            P.tt(V(v4(em), emk), V(v4(e_), ek), V(b4(_ap(mk)), mk.key), ALU.mult)
                        else:
                            em, emk = e_, ek
                        for h in range(4):
                            P.mm(V(C.psb[4 + h][:, 0:65], "pacc%d" % h), V(em[:, h * 128:(h + 1) * 128], emk), V(vp[:, kt, g, :], vk),
                                 start=(i == 0), stop=(i == len(kts) - 1), inc=True)
                    for h in range(4):
                        finish_head(C.psb[4 + h], "pacc%d" % h, g * 4 + h, 1 + br, False)
            P.dma(V(C.ob_d[t0:t0 + 128, :], "ob_d%d" % qt), V(ob[:, :, :].rearrange("p h d -> p (h d)"), "ob"), q="act")
        C.psn = 8


def stage4(P, C, l):
    S = C.S
    with contextlib.ExitStack() as es:
        P.es, old = es, P.es
        sb = P.sb
        nsc, nsh, grow = load_layer_vecs(P, C, l, "m")
        identb = sb("m_identb", [128, 128], BF16)
        P.copy(V(identb[:, :], "identb"), C.ident)
        Woa = sb("m_Woa", [128, 4, D], BF16)
        Wob = sb("m_Wob", [128, 4, D], BF16)
        Wout = sb("m_Wout", [128, 8, D], BF16)
        for h2 in range(2):
            cs_ = slice(h2 * 512, (h2 + 1) * 512)
            P.dma(V(Woa[:, :, cs_], "Woa"), C.w_oa[l, :, cs_].rearrange("(c p) n -> p c n", p=128), q="pool")
            P.dma(V(Wob[:, :, cs_], "Wob"), C.w_ob[l, :, cs_].rearrange("(c p) n -> p c n", p=128), q="pool")
            P.dma(V(Wout[:, :, cs_], "Wout"), C.w_out[l, :, cs_].rearrange("(c p) n -> p c n", p=128), q="pool")
        oab = sb("m_oab", [128, 2, 512])
        oabb = sb("m_oabb", [128, 2, 512], BF16)
        oT = sb("m_oT", [128, 8, 128], BF16)
        mg = sb("m_mg", [128, 2048])
        xt = sb("m_xt", [128, D])
        t1 = sb("m_t1", [128, D])
        t2 = sb("m_t2", [128, D])
        ymb = sb("m_ymb", [128, D], BF16)
        yT = sb("m_yT", [128, 8, 128], BF16)
        xo = sb("m_xo", [128, D])
        for tt in range(S // 128):
            r = slice(tt * 128, (tt + 1) * 128)
            P.dma(V(oab[:, 0, :], "oab"), V(C.oa_d[r, :], "oa_all"))
            P.dma(V(oab[:, 1, :], "oab"), V(C.ob_d[r, :], "ob_all"))
            P.dma(V(mg[:, :], "mg"), V(C.proj_d[r, O_MG:O_MG + 2048], "proj_all"))
            P.dma(V(xt[:, :], "xt"), V(C.xcur[r, :], "x_all"))
            P.copy(V(oabb[:, :, :], "oabb"), V(oab[:, :, :], "oab"), e="pool")
            P.act(V(mg[:, :], "mg"), V(mg[:, :], "mg"), AF.Sigmoid)
            for ab in range(2):
                pb, pk = C.psum()
                for c4 in range(4):
                    P.mm(V(pb[:, c4 * 128:(c4 + 1) * 128], pk), V(oabb[:, ab, c4 * 128:(c4 + 1) * 128], "oabb"), V(identb[:, :], "identb"), inc=(c4 == 3))
                P.copy(V(oT[:, ab * 4:(ab + 1) * 4, :], "oT"), V(pb[:, :].rearrange("p (c t) -> p c t", c=4), pk), e="act")
            for ab in range(2):
                Wm, wk = (Woa, "Woa") if ab == 0 else (Wob, "Wob")
                tdst = t1 if ab == 0 else t2
                for h2 in range(2):
                    pb, pk = C.psum()
                    for k in range(4):
                        P.mm(V(pb[:, :], pk), V(oT[:, ab * 4 + k, :], "oT"), V(Wm[:, k, h2 * 512:(h2 + 1) * 512], wk), start=(k == 0), stop=(k == 3))
                    P.tt(V(tdst[:, h2 * 512:(h2 + 1) * 512], "t%d" % ab), V(pb[:, :], pk),
                         V(mg[:, ab * 1024 + h2 * 512:ab * 1024 + (h2 + 1) * 512], "mg"), ALU.mult)
            P.tt(V(ymb[:, :], "ymb"), V(t1[:, :], "t0"), V(t2[:, :], "t1"), ALU.add, e="pool")
            for half in range(2):
                pb, pk = C.psum()
                for c4 in range(4):
                    c = half * 4 + c4
                    P.mm(V(pb[:, c4 * 128:(c4 + 1) * 128], pk), V(ymb[:, c * 128:(c + 1) * 128], "ymb"), V(identb[:, :], "identb"), inc=(c4 == 3))
                P.copy(V(yT[:, half * 4:(half + 1) * 4, :], "yT"), V(pb[:, :].rearrange("p (c t) -> p c t", c=4), pk), e="act")
            for h2 in range(2):
                pb, pk = C.psum()
                for k in range(8):
                    P.mm(V(pb[:, :], pk), V(yT[:, k, :], "yT"), V(Wout[:, k, h2 * 512:(h2 + 1) * 512], "Wout"), start=(k == 0), stop=(k == 7))
                P.tt(V(xo[:, h2 * 512:(h2 + 1) * 512], "xo"), V(pb[:, :], pk), V(_ap(grow)[:, h2 * 512:(h2 + 1) * 512], grow.key), ALU.mult)
            P.tt(V(xo[:, :], "xo"), V(xo[:, :], "xo"), V(xt[:, :], "xt"), ALU.add, e="pool")
            P.dma(V(C.xmid_d[r, :], "xmid%d" % tt), V(xo[:, :], "xo"), q="act")
        P.barrier()
        P.es = old


def stage5(P, C, l, xout):
    S = C.S
    moe = (l % 2 == 1)
    li = l // 2
    TS = min(8, S // 128)
    T = TS * 128
    NH = (T + 511) // 512
    HW = min(512, T)
    with contextlib.ExitStack() as es:
        P.es, old = es, P.es
        sb = P.sb
        nsc, nsh, grow = load_layer_vecs(P, C, l, "f")
        C.p_ss = Pool(P, "f_ss", 2, [128, 1])
        C.p_sq = Pool(P, "f_sq", 2, [128, D])
        xp = Pool(P, "f_x", 2, [128, D])
        hT = sb("f_hT", [128, 8, T], BF16)
        acc = sb("f_acc", [128, TS, D])
        gT = sb("f_gT", [128, 4, T], BF16)
        sa = sb("f_sa", [128, 512])
        w1p = Pool(P, "f_w1", 2, [128, 8, 512], BF16)
        w3p = Pool(P, "f_w3", 2, [128, 8, 512], BF16)
        w2p = Pool(P, "f_w2", 2, [128, 4, D], BF16)
        xo = sb("f_xo", [128, D])
        if moe:
            Wr = sb("f_Wr", [128, 8, NEXP])
            P.dma(V(Wr[:, :, :], "Wr"), C.w_router[li].rearrange("(c p) e -> p c e", p=128))
            h32 = sb("f_h32", [128, 8, 128])
            lg = sb("f_lg", [128, NEXP])
            m8 = sb("f_m8", [128, 8])
            nt1 = sb("f_nt1", [128, 1])
            msk = sb("f_msk", [128, NEXP])
            ex = sb("f_ex", [128, NEXP])
            den = sb("f_den", [128, 1])
            comb = sb("f_comb", [128, TS, NEXP])
        nexp = NEXP if moe else 1
        for st in range(S // T):
            for sub in range(TS):
                tt = st * TS + sub
                xt, kx = xp.next()
                P.dma(V(xt[:, :], kx), V(C.xmid_d[tt * 128:(tt + 1) * 128, :], "xmid_all"))
                norm_mod_T(P, C, xt, kx, nsc, nsh, hT, "hT", tok0=sub * 128, hT32=((h32, "h32") if moe else None))
                if moe:
                    pb, pk = C.psum()
                    for k in range(8):
                        P.mm(V(pb[:, 0:NEXP], pk), V(h32[:, k, :], "h32"), V(Wr[:, k, :], "Wr"), start=(k == 0), stop=(k == 7))
                    P.copy(V(lg[:, :], "lg"), V(pb[:, 0:NEXP], pk))
                    P.op("dve", lambda v: v.max(m8[:, :], lg[:, :]), reads=[V(lg[:, :], "lg")], writes=[V(m8[:, :], "m8")])
                    P.ts(V(nt1[:, :], "nt1"), V(m8[:, 0:1], "m8"), -1.0, ALU.mult)
                    P.ts(V(msk[:, :], "msk"), V(lg[:, :], "lg"), V(m8[:, 1:2], "m8"), ALU.is_ge)
                    P.act(V(ex[:, :], "ex"), V(lg[:, :], "lg"), AF.Exp, bias=V(nt1[:, :], "nt1"))
                    P.tt(V(ex[:, :], "ex"), V(ex[:, :], "ex"), V(msk[:, :], "msk"), ALU.mult)
                    P.reduce(V(den[:, :], "den"), V(ex[:, :], "ex"), ALU.add)
                    P.op("dve", lambda v: v.reciprocal(den[:, :], den[:, :]), reads=[V(den[:, :], "den")], writes=[V(den[:, :], "den")])
                    P.ts(V(comb[:, sub, :], "comb"), V(ex[:, :], "ex"), V(den[:, 0:1], "den"), ALU.mult)
            for e in range(nexp):
                for j in range(FFN // 512):
                    fs = slice(j * 512, (j + 1) * 512)
                    w1, k1 = w1p.next()
                    w3, k3 = w3p.next()
                    w2, k2 = w2p.next()
                    if moe:
                        s1, s3, s2 = C.w1_moe[li, e, :, fs], C.w3_moe[li, e, :, fs], C.w2_moe[li, e, fs, :]
                    else:
                        s1, s3, s2 = C.w1_dense[li, :, fs], C.w3_dense[li, :, fs], C.w2_dense[li, fs, :]
                    P.dma(V(w1[:, :, :], k1), s1.rearrange("(c p) n -> p c n", p=128), q="pool")
                    P.dma(V(w3[:, :, :], k3), s3.rearrange("(c p) n -> p c n", p=128), q="pool")
                    for h2 in range(2):
                        P.dma(V(w2[:, :, h2 * 512:(h2 + 1) * 512], k2), s2[:, h2 * 512:(h2 + 1) * 512].rearrange("(c p) n -> p c n", p=128), q="pool")
                    for i in range(4):
                        for nh in range(NH):
                            ts_ = slice(nh * 512, nh * 512 + HW)
                            pa, pak = C.psum()
                            for k in range(8):
                                P.mm(V(pa[:, 0:HW], pak), V(w1[:, k, i * 128:(i + 1) * 128], k1), V(hT[:, k, ts_], "hT"), start=(k == 0), stop=(k == 7))
                            pb, pbk = C.psum()
                            for k in range(8):
                                P.mm(V(pb[:, 0:HW], pbk), V(w3[:, k, i * 128:(i + 1) * 128], k3), V(hT[:, k, ts_], "hT"), start=(k == 0), stop=(k == 7))
                            P.act(V(sa[:, 0:HW], "sa"), V(pa[:, 0:HW], pak), AF.Silu)
                            P.tt(V(gT[:, i, ts_], "gT"), V(pb[:, 0:HW], pbk), V(sa[:, 0:HW], "sa"), ALU.mult)
                    first = (e == 0 and j == 0)
                    for sub in range(TS):
                        for h2 in range(2):
                            po, pok = C.psum()
                            for i in range(4):
                                P.mm(V(po[:, :], pok), V(gT[:, i, sub * 128:(sub + 1) * 128], "gT"), V(w2[:, i, h2 * 512:(h2 + 1) * 512], k2), start=(i == 0), stop=(i == 3))
                            a_ = V(acc[:, sub, h2 * 512:(h2 + 1) * 512], "acc")
                            if moe:
                                cb = V(comb[:, sub, e:e + 1], "comb")
                                if first:
                                    P.ts(a_, V(po[:, :], pok), cb, ALU.mult)
                                else:
                                    P.stt(a_, V(po[:, :], pok), cb, a_, ALU.mult, ALU.add)
                            else:
                                if first:
                                    P.copy(a_, V(po[:, :], pok))
                                else:
                                    P.tt(a_, V(po[:, :], pok), a_, ALU.add)
            for sub in range(TS):
                tt = st * TS + sub
                xt, kx = xp.next()
                P.dma(V(xt[:, :], kx), V(C.xmid_d[tt * 128:(tt + 1) * 128, :], "xmid_all"))
                P.tt(V(xo[:, :], "xo"), V(acc[:, sub, :], "acc"), grow, ALU.mult)
                P.tt(V(xo[:, :], "xo"), V(xo[:, :], "xo"), V(xt[:, :], kx), ALU.add, e="pool")
                P.dma(V(xout[tt * 128:(tt + 1) * 128, :], "xout%d" % tt), V(xo[:, :], "xo"), q="act")
        P.barrier()
        P.es = old


_PROG_CACHE = {}


def kernel(**inputs):
    x = np.asarray(inputs["x"])
    B, S, _ = x.shape
    L = int(np.asarray(inputs["w_ada"]).shape[0])
    key = (S, L)
    if key not in _PROG_CACHE:
        _PROG_CACHE[key] = build_program(S, L)
    nc = _PROG_CACHE[key]
    consts = host_consts(S)
    shared = {}
    for k, v in inputs.items():
        if k in ("x", "c", "positions"):
            continue
        shared[k] = np.ascontiguousarray(np.asarray(v))
    shared.update(consts)
    in_maps = []
    for b in range(B):
        m = dict(shared)
        m["x"] = np.ascontiguousarray(x[b])
        m["c"] = np.ascontiguousarray(np.asarray(inputs["c"])[b])
        m["positions"] = np.ascontiguousarray(np.asarray(inputs["positions"])[b].astype(np.int32))
        in_maps.append(m)
    res = run_bass_kernel_spmd(nc, in_maps, core_ids=list(range(B)))
    return np.stack([np.asarray(r["out"]) for r in res.results], axis=0).astype(np.float32)
```
